# Optimizing a Trainium2 kernel written in Bass

```python
import math
import jax, jax.numpy as jnp
from jax import lax
import numpy as np

D_MODEL = 1024
BATCH = 4
SEQ = 8192
DEPTH = 2

HEAD_DIM = 64
D_MIX = D_MODEL
N_MIXERS = 4
N_GROUP_HEADS = D_MIX // (N_MIXERS * HEAD_DIM)
QB = 128
NEG = -1e30
TINY = 1e-30
BIG = 1e9
RMS_EPS = 1e-6

SWA_HEADS = N_GROUP_HEADS
SWA_KV_HEADS = 2
SWA_WINDOW = 128
MLA_HEADS = N_GROUP_HEADS
MLA_Q_RANK = 3 * D_MODEL // 16
MLA_KV_RANK = D_MODEL // 8
MLA_NOPE = 64
MLA_ROPE = 32
MLA_V = 64
ROPE_THETA = 10000.0
NSA_HEADS = N_GROUP_HEADS
NSA_CMP_LEN = 32
NSA_CMP_STRIDE = 16
NSA_CMP_HIDDEN = 128
NSA_SEL_BLOCK = 64
NSA_TOPN = 16
NSA_WINDOW = 512
NSA_N_BRANCH = 3
MOBA_HEADS = N_GROUP_HEADS
MOBA_BLOCK = 256
MOBA_TOPK = 3
D_FF = 4 * D_MODEL
N_ALIBI = SWA_HEADS + NSA_HEADS + MOBA_HEADS
N_ADA = 6

SWA_COLS = (SWA_HEADS + 2 * SWA_KV_HEADS) * HEAD_DIM
MLA_COLS = MLA_Q_RANK + MLA_KV_RANK + MLA_ROPE
NSA_COLS = NSA_HEADS * HEAD_DIM + 2 * NSA_N_BRANCH * HEAD_DIM + NSA_N_BRANCH * NSA_HEADS
MOBA_COLS = 3 * MOBA_HEADS * HEAD_DIM
D_IN = SWA_COLS + MLA_COLS + NSA_COLS + MOBA_COLS

kernel_name = 'hybrid_parallel_heads_swa_mla_nsa_moba'


def alibi_slopes():
    s = np.array([2.0 ** (-8.0 * (i + 1) / N_ALIBI) for i in range(N_ALIBI)], np.float32)
    return jnp.asarray(s[0::3]), jnp.asarray(s[1::3]), jnp.asarray(s[2::3])


def split_last(a, sizes):
    offs = np.cumsum([0] + list(sizes))
    return [a[..., int(offs[i]):int(offs[i + 1])] for i in range(len(sizes))]


def heads(t, n):
    B, S, W = t.shape
    return t.reshape(B, S, n, W // n).transpose(0, 2, 1, 3)


def merge_heads(t):
    B, H, S, d = t.shape
    return t.transpose(0, 2, 1, 3).reshape(B, S, H * d)


def unblock(o):
    nq, B, H, qb, d = o.shape
    return o.transpose(1, 2, 0, 3, 4).reshape(B, H, nq * qb, d)


def rmsnorm(x, g):
    xf = x.astype(jnp.float32)
    y = xf * lax.rsqrt(jnp.mean(xf * xf, axis=-1, keepdims=True) + RMS_EPS)
    return (y * g.astype(jnp.float32)).astype(x.dtype)


def rope(x, pos):
    half = x.shape[-1] // 2
    freqs = ROPE_THETA ** (-jnp.arange(half, dtype=jnp.float32) / half)
    ang = pos[:, None] * freqs[None, :]
    cos, sin = jnp.cos(ang), jnp.sin(ang)
    x1 = x[..., :half].astype(jnp.float32)
    x2 = x[..., half:].astype(jnp.float32)
    return jnp.concatenate([x1 * cos - x2 * sin, x1 * sin + x2 * cos], axis=-1).astype(x.dtype)


def masked_softmax(s, mask, sink=None):
    s = jnp.where(mask, s.astype(jnp.float32), NEG)
    m = jnp.max(s, axis=-1, keepdims=True)
    if sink is not None:
        m = jnp.maximum(m, sink)
    e = jnp.where(mask, jnp.exp(s - m), 0.0)
    den = jnp.sum(e, axis=-1, keepdims=True)
    if sink is not None:
        den = den + jnp.exp(sink - m)
    return e / jnp.maximum(den, TINY)


def banded_attention(q, k, v, window, slopes, sink=None):
    B, KVH, G, S, d = q.shape
    nq = S // QB
    nb = window // QB
    kw = (nb + 1) * QB

    def band(t):
        tb = t.reshape(B, KVH, nq, QB, d)
        tp = jnp.pad(tb, ((0, 0), (0, 0), (nb, 0), (0, 0), (0, 0)))
        return jnp.concatenate([tp[:, :, i:i + nq] for i in range(nb + 1)], axis=3)

    kb, vb = band(k), band(v)
    qb = q.reshape(B, KVH, G, nq, QB, d)
    s = jnp.einsum('bkgnqd,bknsd->bkgnqs', qb, kb) * (d ** -0.5)
    dist = nb * QB + jnp.arange(QB)[:, None] - jnp.arange(kw)[None, :]
    key_pos = (jnp.arange(nq)[:, None] - nb) * QB + jnp.arange(kw)[None, :]
    mask = ((dist >= 0) & (dist < window))[None, :, :] & (key_pos >= 0)[:, None, :]
    bias = -slopes.astype(jnp.float32)[:, :, None, None, None] * dist
    sink_b = None if sink is None else sink.astype(jnp.float32)[None, :, :, None, None, None]
    p = masked_softmax(s.astype(jnp.float32) + bias, mask, sink_b).astype(v.dtype)
    o = jnp.einsum('bkgnqs,bknsd->bkgnqd', p, vb)
    return o.reshape(B, KVH * G, S, d)


def causal_attention_blocks(q, k, v, scale):
    B, H, S, dq = q.shape
    nq = S // QB
    qb = q.reshape(B, H, nq, QB, dq).transpose(2, 0, 1, 3, 4)
    kpos = jnp.arange(S)

    def one(args):
        qi, n = args
        tq = n * QB + jnp.arange(QB)
        s = jnp.einsum('bhqd,bhsd->bhqs', qi, k) * scale
        p = masked_softmax(s, kpos[None, :] <= tq[:, None]).astype(v.dtype)
        return jnp.einsum('bhqs,bhsd->bhqd', p, v)

    return unblock(lax.map(one, (qb, jnp.arange(nq))))


def swa_mixer(z, sinks, slopes):
    G = SWA_HEADS // SWA_KV_HEADS
    q, k, v = split_last(z, [SWA_HEADS * HEAD_DIM, SWA_KV_HEADS * HEAD_DIM, SWA_KV_HEADS * HEAD_DIM])
    q = heads(q, SWA_HEADS)
    B, _, S, d = q.shape
    q = q.reshape(B, SWA_KV_HEADS, G, S, d)
    o = banded_attention(q, heads(k, SWA_KV_HEADS), heads(v, SWA_KV_HEADS), SWA_WINDOW,
                         slopes.reshape(SWA_KV_HEADS, G), sinks.reshape(SWA_KV_HEADS, G))
    return merge_heads(o)


def mla_mixer(z, q_norm_g, kv_norm_g, w_uq, w_ukv):
    cq, ckv, k_pe = split_last(z, [MLA_Q_RANK, MLA_KV_RANK, MLA_ROPE])
    B, S, _ = z.shape
    pos = jnp.arange(S, dtype=jnp.float32)
    q = heads(rmsnorm(cq, q_norm_g) @ w_uq, MLA_HEADS)
    kv = heads(rmsnorm(ckv, kv_norm_g) @ w_ukv, MLA_HEADS)
    q = jnp.concatenate([q[..., :MLA_NOPE], rope(q[..., MLA_NOPE:], pos)], axis=-1)
    k_pe = jnp.broadcast_to(rope(k_pe, pos)[:, None], (B, MLA_HEADS, S, MLA_ROPE))
    k = jnp.concatenate([kv[..., :MLA_NOPE], k_pe], axis=-1)
    v = kv[..., MLA_NOPE:]
    o = causal_attention_blocks(q, k, v, (MLA_NOPE + MLA_ROPE) ** -0.5)
    return merge_heads(o)


def nsa_mixer(z, slopes, pos_k, pos_v, ck_w1, ck_w2, cv_w1, cv_w2):
    d, H = HEAD_DIM, NSA_HEADS
    q, kc, vc, ks, vs, kw, vw, gl = split_last(z, [H * d, d, d, d, d, d, d, NSA_N_BRANCH * H])
    B, S, _ = z.shape
    q = heads(q, H)
    scale = d ** -0.5
    ratio = NSA_CMP_LEN // NSA_CMP_STRIDE
    n_cmp = S // NSA_CMP_STRIDE - ratio + 1

    def compress(t, pos_emb, w1, w2):
        ch = t.reshape(B, S // NSA_CMP_STRIDE, NSA_CMP_STRIDE, d)
        blk = jnp.concatenate([ch[:, i:i + n_cmp] for i in range(ratio)], axis=2) + pos_emb
        return jax.nn.gelu(blk.reshape(B, n_cmp, NSA_CMP_LEN * d) @ w1) @ w2

    k_cmp = compress(kc, pos_k, ck_w1, ck_w2)
    v_cmp = compress(vc, pos_v, cv_w1, cv_w2)
    cmp_end = jnp.arange(n_cmp) * NSA_CMP_STRIDE + NSA_CMP_LEN - 1
    n_sel = S // NSA_SEL_BLOCK
    topn = min(NSA_TOPN, n_sel)
    sel_ratio = NSA_SEL_BLOCK // NSA_CMP_STRIDE
    n_off = sel_ratio + ratio - 1
    k_sel = ks.reshape(B, n_sel, NSA_SEL_BLOCK, d)
    v_sel = vs.reshape(B, n_sel, NSA_SEL_BLOCK, d)
    nq = S // QB
    qb = q.reshape(B, H, nq, QB, d).transpose(2, 0, 1, 3, 4)
    m = slopes.astype(jnp.float32)[None, :, None, None]
    blk_ids = jnp.arange(n_sel)
    bi = jnp.arange(B)[:, None, None]
    nk = topn * NSA_SEL_BLOCK

    def one(args):
        qi, n = args
        tq = n * QB + jnp.arange(QB)
        dist_c = tq[:, None] - cmp_end[None, :]
        s_c = jnp.einsum('bhqd,bcd->bhqc', qi, k_cmp) * scale
        p_c = masked_softmax(s_c.astype(jnp.float32) - m * dist_c, dist_c >= 0)
        o_c = jnp.einsum('bhqc,bcd->bhqd', p_c.astype(v_cmp.dtype), v_cmp)
        pp = jnp.pad(p_c.sum(axis=1), ((0, 0), (0, 0), (ratio - 1, sel_ratio)))
        imp = sum(pp[..., o::sel_ratio][..., :n_sel] for o in range(n_off))
        cur = tq // NSA_SEL_BLOCK
        cand = blk_ids[None, :] <= cur[:, None]
        forced = (blk_ids[None, :] == 0) | (blk_ids[None, :] == cur[:, None]) | (blk_ids[None, :] == cur[:, None] - 1)
        imp = jnp.where(cand, jnp.where(forced, BIG, imp), NEG)
        _, idx = lax.top_k(imp, topn)
        valid = jnp.take_along_axis(jnp.broadcast_to(cand, imp.shape), idx, axis=-1)
        kg = k_sel[bi, idx].reshape(B, QB, nk, d)
        vg = v_sel[bi, idx].reshape(B, QB, nk, d)
        key_pos = (idx[..., None] * NSA_SEL_BLOCK + jnp.arange(NSA_SEL_BLOCK)).reshape(B, QB, nk)
        dist_s = tq[None, :, None] - key_pos
        mask_s = (jnp.repeat(valid, NSA_SEL_BLOCK, axis=-1) & (dist_s >= 0))[:, None]
        s_s = jnp.einsum('bhqd,bqkd->bhqk', qi, kg) * scale
        p_s = masked_softmax(s_s.astype(jnp.float32) - m * dist_s[:, None], mask_s)
        o_s = jnp.einsum('bhqk,bqkd->bhqd', p_s.astype(vg.dtype), vg)
        return o_c, o_s

    o_c, o_s = lax.map(one, (qb, jnp.arange(nq)))
    o_c, o_s = unblock(o_c), unblock(o_s)
    o_w = banded_attention(q[:, None], heads(kw, 1), heads(vw, 1), NSA_WINDOW, slopes[None, :], None)
    g = jax.nn.sigmoid(gl.astype(jnp.float32)).reshape(B, S, NSA_N_BRANCH, H).transpose(0, 3, 1, 2).astype(q.dtype)
    o = g[..., 0:1] * o_c + g[..., 1:2] * o_s + g[..., 2:3] * o_w
    return merge_heads(o)


def moba_mixer(z, slopes):
    d, H = HEAD_DIM, MOBA_HEADS
    q, k, v = [heads(t, H) for t in split_last(z, [H * d] * 3)]
    B, _, S, _ = q.shape
    scale = d ** -0.5
    n_blk = -(-S // MOBA_BLOCK)
    pad = n_blk * MOBA_BLOCK - S
    kp = jnp.pad(k, ((0, 0), (0, 0), (0, pad), (0, 0)))
    vp = jnp.pad(v, ((0, 0), (0, 0), (0, pad), (0, 0)))
    k_blk = kp.reshape(B, H, n_blk, MOBA_BLOCK, d)
    v_blk = vp.reshape(B, H, n_blk, MOBA_BLOCK, d)
    k_mean = jnp.mean(k_blk, axis=3)
    topk = min(MOBA_TOPK, n_blk)
    nq = S // QB
    qb = q.reshape(B, H, nq, QB, d).transpose(2, 0, 1, 3, 4)
    m = slopes.astype(jnp.float32)[None, :, None, None]
    bi = jnp.arange(B)[:, None, None, None]
    hi = jnp.arange(H)[None, :, None, None]
    blk_ids = jnp.arange(n_blk)
    ng = topk * MOBA_BLOCK

    def one(args):
        qi, n = args
        tq = n * QB + jnp.arange(QB)
        cur = (n * QB) // MOBA_BLOCK
        gate = jnp.einsum('bhqd,bhjd->bhqj', qi, k_mean).astype(jnp.float32)
        gate = jnp.where(blk_ids < cur, gate, NEG)
        _, idx = lax.top_k(gate, topk)
        valid = idx < cur
        kg = k_blk[bi, hi, idx].reshape(B, H, QB, ng, d)
        vg = v_blk[bi, hi, idx].reshape(B, H, QB, ng, d)
        key_pos = (idx[..., None] * MOBA_BLOCK + jnp.arange(MOBA_BLOCK)).reshape(B, H, QB, ng)
        s_g = jnp.einsum('bhqd,bhqkd->bhqk', qi, kg) * scale - m * (tq[:, None] - key_pos)
        mask_g = jnp.repeat(valid, MOBA_BLOCK, axis=-1)
        start = cur * MOBA_BLOCK
        k_own = lax.dynamic_slice_in_dim(kp, start, MOBA_BLOCK, axis=2)
        v_own = lax.dynamic_slice_in_dim(vp, start, MOBA_BLOCK, axis=2)
        dist_own = tq[:, None] - (start + jnp.arange(MOBA_BLOCK))[None, :]
        s_o = jnp.einsum('bhqd,bhsd->bhqs', qi, k_own) * scale - m * dist_own
        mask_o = jnp.broadcast_to(dist_own >= 0, s_o.shape)
        p = masked_softmax(jnp.concatenate([s_g, s_o], axis=-1),
                           jnp.concatenate([mask_g, mask_o], axis=-1)).astype(v.dtype)
        return (jnp.einsum('bhqk,bhqkd->bhqd', p[..., :ng], vg)
                + jnp.einsum('bhqs,bhsd->bhqd', p[..., ng:], v_own))

    return merge_heads(unblock(lax.map(one, (qb, jnp.arange(nq)))))


def setup_inputs(seed: int = 0) -> dict:
    key = jax.random.key(seed)
    ks = jax.random.split(key, 24)
    f32 = jnp.float32

    def nrm(k, shape, s):
        return jax.random.normal(k, shape, f32) * s

    return {
        'x': nrm(ks[0], (BATCH, SEQ, D_MODEL), 1.0),
        'c': nrm(ks[1], (BATCH, D_MODEL), 1.0),
        'norm_mix_g': 1.0 + nrm(ks[2], (DEPTH, D_MODEL), 0.02),
        'norm_mlp_g': 1.0 + nrm(ks[3], (DEPTH, D_MODEL), 0.02),
        'w_ada': nrm(ks[4], (DEPTH, D_MODEL, N_ADA * D_MODEL), 0.5 * D_MODEL ** -0.5),
        'b_ada': nrm(ks[5], (DEPTH, N_ADA * D_MODEL), 0.01),
        'w_in': nrm(ks[6], (DEPTH, D_MODEL, D_IN), D_MODEL ** -0.5),
        'w_out': nrm(ks[7], (DEPTH, D_MIX, D_MODEL), D_MIX ** -0.5),
        'swa_sinks': nrm(ks[8], (DEPTH, SWA_HEADS), 1.0),
        'mla_q_norm_g': 1.0 + nrm(ks[9], (DEPTH, MLA_Q_RANK), 0.02),
        'mla_kv_norm_g': 1.0 + nrm(ks[10], (DEPTH, MLA_KV_RANK), 0.02),
        'mla_w_uq': nrm(ks[11], (DEPTH, MLA_Q_RANK, MLA_HEADS * (MLA_NOPE + MLA_ROPE)), MLA_Q_RANK ** -0.5),
        'mla_w_ukv': nrm(ks[12], (DEPTH, MLA_KV_RANK, MLA_HEADS * (MLA_NOPE + MLA_V)), MLA_KV_RANK ** -0.5),
        'nsa_cmp_pos_k': nrm(ks[13], (DEPTH, NSA_CMP_LEN, HEAD_DIM), 0.02),
        'nsa_cmp_pos_v': nrm(ks[14], (DEPTH, NSA_CMP_LEN, HEAD_DIM), 0.02),
        'nsa_cmp_k_w1': nrm(ks[15], (DEPTH, NSA_CMP_LEN * HEAD_DIM, NSA_CMP_HIDDEN), (NSA_CMP_LEN * HEAD_DIM) ** -0.5),
        'nsa_cmp_k_w2': nrm(ks[16], (DEPTH, NSA_CMP_HIDDEN, HEAD_DIM), NSA_CMP_HIDDEN ** -0.5),
        'nsa_cmp_v_w1': nrm(ks[17], (DEPTH, NSA_CMP_LEN * HEAD_DIM, NSA_CMP_HIDDEN), (NSA_CMP_LEN * HEAD_DIM) ** -0.5),
        'nsa_cmp_v_w2': nrm(ks[18], (DEPTH, NSA_CMP_HIDDEN, HEAD_DIM), NSA_CMP_HIDDEN ** -0.5),
        'w_up': nrm(ks[19], (DEPTH, D_MODEL, D_FF), D_MODEL ** -0.5),
        'w_down': nrm(ks[20], (DEPTH, D_FF, D_MODEL), D_FF ** -0.5),
        'final_norm_g': 1.0 + nrm(ks[21], (D_MODEL,), 0.02),
    }


def reference(x, c, norm_mix_g, norm_mlp_g, w_ada, b_ada, w_in, w_out, swa_sinks,
              mla_q_norm_g, mla_kv_norm_g, mla_w_uq, mla_w_ukv,
              nsa_cmp_pos_k, nsa_cmp_pos_v, nsa_cmp_k_w1, nsa_cmp_k_w2, nsa_cmp_v_w1, nsa_cmp_v_w2,
              w_up, w_down, final_norm_g):
    slopes_a, slopes_c, slopes_d = alibi_slopes()
    c_act = jax.nn.silu(c)
    for l in range(DEPTH):
        mod = c_act @ w_ada[l] + b_ada[l]
        sh1, sc1, g1, sh2, sc2, g2 = [t[:, None, :] for t in jnp.split(mod, N_ADA, axis=-1)]
        h = rmsnorm(x, norm_mix_g[l]) * (1.0 + sc1) + sh1
        z = h @ w_in[l]
        za, zb, zc, zd = split_last(z, [SWA_COLS, MLA_COLS, NSA_COLS, MOBA_COLS])
        o = jnp.concatenate([
            swa_mixer(za, swa_sinks[l], slopes_a),
            mla_mixer(zb, mla_q_norm_g[l], mla_kv_norm_g[l], mla_w_uq[l], mla_w_ukv[l]),
            nsa_mixer(zc, slopes_c, nsa_cmp_pos_k[l], nsa_cmp_pos_v[l],
                      nsa_cmp_k_w1[l], nsa_cmp_k_w2[l], nsa_cmp_v_w1[l], nsa_cmp_v_w2[l]),
            moba_mixer(zd, slopes_d),
        ], axis=-1)
        x = x + g1 * (o @ w_out[l])
        h = rmsnorm(x, norm_mlp_g[l]) * (1.0 + sc2) + sh2
        x = x + g2 * (jnp.square(jax.nn.relu(h @ w_up[l])) @ w_down[l])
    return rmsnorm(x, final_norm_g)
```

```python
import math
from contextlib import ExitStack

import numpy as np
import ml_dtypes

import concourse.bass as bass
import concourse.mybir as mybir
from concourse.bass_utils import run_bass_kernel_spmd

F32 = mybir.dt.float32
BF16 = mybir.dt.bfloat16
AF = mybir.ActivationFunctionType
ALU = mybir.AluOpType
AX = mybir.AxisListType

D = 1024
HD = 64
DFF = 4096
DIN = 2284
NEGB = -30000.0
BIGV = 1.0e9
NEGV = -1.0e30
EPS = 1e-6
N_ALIBI = 12
_sl = np.array([2.0 ** (-8.0 * (i + 1) / N_ALIBI) for i in range(N_ALIBI)], np.float32)
SLOPES_A, SLOPES_C, SLOPES_D = _sl[0::3], _sl[1::3], _sl[2::3]


class Buf:
    __slots__ = ("w", "r", "name")

    def __init__(self, name=""):
        self.w = None
        self.r = {}
        self.name = name


class _Eng:
    def __init__(self, name, h, sem, self_sync):
        self.name, self.h, self.sem, self.n = name, h, sem, 0
        self.seen = {}
        self.self_sync = self_sync


class Sched:
    ND = 8

    def __init__(self, nc):
        self.nc = nc
        self.e = {}
        for name, h, ss in (("pe", nc.tensor, False), ("act", nc.scalar, True), ("dve", nc.vector, True),
                            ("pool", nc.gpsimd, True), ("sp", nc.sync, True)):
            self.e[name] = _Eng(name, h, nc.alloc_semaphore("s_" + name), ss)
        self.dpool = {q: [nc.alloc_semaphore(f"d_{q}{i}") for i in range(self.ND)] for q in ("sp", "pool", "act")}
        self.dcnt = {q: 0 for q in self.dpool}
        self.ninst = 0
        import os
        self.maxops = int(os.environ.get("KMAX", "1000000000"))
        self.count = 0

    def _waits(self, E, reads, writes, extra=()):
        need = {}

        def add(tok):
            if tok is None:
                return
            s, v = tok
            if need.get(s.num, (None, 0))[1] < v:
                need[s.num] = (s, v)
        for b in reads:
            add(b.w)
        for b in writes:
            add(b.w)
            for t in b.r.values():
                add(t)
        for t in extra:
            add(t)
        for num, (s, v) in need.items():
            if num == E.sem.num and not E.self_sync:
                continue
            if E.seen.get(num, 0) < v:
                E.h.wait_ge(s, v)
                E.seen[num] = v
                self.ninst += 1

    def _mark(self, tok, reads, writes):
        for b in reads:
            b.r[tok[0].num] = tok
        for b in writes:
            b.w = tok
            b.r = {}

    def op(self, en, fn, reads=(), writes=()):
        self.count += 1
        if self.count > self.maxops:
            return
        E = self.e[en]
        self._waits(E, reads, writes)
        inst = fn(E.h)
        E.n += 1
        inst.then_inc(E.sem, 1)
        self.ninst += 1
        self._mark((E.sem, E.n), reads, writes)

    def dma(self, q, out, in_, reads=(), writes=(), **kw):
        self.count += 1
        if self.count > self.maxops:
            return
        E = self.e[q]
        i = self.dcnt[q]
        self.dcnt[q] += 1
        sem = self.dpool[q][i % self.ND]
        val = 16 * (i // self.ND + 1)
        extra = [(sem, val - 16)] if i >= self.ND else []
        self._waits(E, reads, writes, extra)
        E.h.dma_start(out=out, in_=in_, **kw).then_inc(sem, 16)
        self.ninst += 1
        self._mark((sem, val), reads, writes)

    def barrier(self):
        toks = [(E.sem, E.n) for E in self.e.values() if E.n > 0]
        for q, pool in self.dpool.items():
            n = self.dcnt[q]
            for k, s in enumerate(pool):
                cnt = (n - k + self.ND - 1) // self.ND if n > k else 0
                if cnt > 0:
                    toks.append((s, 16 * cnt))
        for E in self.e.values():
            for s, v in toks:
                if s.num == E.sem.num:
                    continue
                if E.seen.get(s.num, 0) < v:
                    E.h.wait_ge(s, v)
                    E.seen[s.num] = v
                    self.ninst += 1

    def final_wait(self, bufs):
        E = self.e["sp"]
        self._waits(E, bufs, ())


def _bf(a):
    return np.asarray(a, np.float32).astype(ml_dtypes.bfloat16)


def make_consts(S):
    c = {}
    c["ident"] = _bf(np.eye(128))
    pos = np.arange(S)
    kr = np.stack([pos % 128, pos // 128, np.ones(S), np.ones(S), np.ones(S)]).astype(np.float64)
    c["krows"] = _bf(kr)
    ncmp = S // 16 - 1
    ce = np.arange(512) * 16 + 31
    krc = np.stack([ce % 128, ce // 128, np.ones(512), np.ones(512), np.ones(512)]).astype(np.float64)
    krc[:, ncmp:] = 0.0
    c["krows_c"] = _bf(krc)
    qr = np.zeros((12, 5, S), np.float64)
    for i, s in enumerate(np.concatenate([SLOPES_A, SLOPES_C, SLOPES_D])):
        sr = float(np.float32(_bf(s)))
        v = -sr * pos.astype(np.float64)
        t1 = _bf(v).astype(np.float64)
        t2 = _bf(v - t1).astype(np.float64)
        t3 = _bf(v - t1 - t2).astype(np.float64)
        assert np.all(t1 + t2 + t3 == v)
        qr[i] = np.stack([np.full(S, sr), np.full(S, 128.0 * sr), t1, t2, t3])
    c["qrows"] = _bf(qr)
    k = np.arange(128)[:, None]
    q = np.arange(128)[None, :]
    diag = np.where(k <= q, 0.0, NEGB)
    win = np.where(k > q, 0.0, NEGB)
    c["mb_diag4"] = _bf(np.tile(diag, (1, 4)))
    c["mb_win4"] = _bf(np.tile(win, (1, 4)))
    grp = np.zeros((128, 4, 4, 128))
    for r in range(4):
        for j in range(4):
            grp[:, r, j, :] = NEGB if j < r else (diag if j == r else 0.0)
    c["mb_grp"] = _bf(grp.reshape(128, 4, 512))
    cl = np.arange(128)[:, None, None]
    dl = np.arange(17)[None, :, None]
    qq = np.arange(128)[None, None, :]
    c["mb_cmp"] = _bf(np.where(16 * cl + 31 - qq <= 128 * dl, 0.0, NEGB))
    cc = np.arange(512)[:, None]
    jj = np.arange(128)[None, :]
    M = ((cc >= 4 * jj - 1) & (cc <= 4 * jj + 3) & (cc < ncmp)).astype(np.float64)
    c["mpool"] = _bf(M.reshape(4, 128, 128).transpose(1, 0, 2))
    ones_c = np.ones((512,))
    ones_c[ncmp:] = 0.0
    c["ones_c"] = _bf(ones_c.reshape(4, 128).T.copy())
    kk = np.arange(S)[None, :]
    c["g_c"] = _bf((kk // 64 == np.arange(128)[:, None]).astype(np.float64))
    c["g_d"] = _bf((kk // 256 == np.arange(128)[:, None]).astype(np.float64))
    ql = np.arange(128)[:, None]
    hi = (ql >= 64).astype(np.int64)
    u = np.arange(254)[None, :] - 126
    cand = u <= hi
    forced = (u == hi) | (u == hi - 1)
    c["nsa_A"] = (cand & ~forced).astype(np.float32)
    c["nsa_B"] = np.where(~cand, NEGV, np.where(forced, BIGV, 0.0)).astype(np.float32)
    u2 = np.arange(64)[None, :] - 32
    c["moba_m"] = np.broadcast_to(np.where(u2 < 0, 0.0, NEGV), (128, 64)).astype(np.float32).copy()
    half = 16
    freqs = (10000.0 ** (-np.arange(half, dtype=np.float32) / half)).astype(np.float32)
    ang = pos.astype(np.float32)[:, None] * freqs[None, :]
    cos, sin = np.cos(ang).astype(np.float32), np.sin(ang).astype(np.float32)
    C = np.concatenate([cos, cos], 1)
    Sg = np.concatenate([-sin, sin], 1)
    NT = S // 128
    c["rope_c"] = C.reshape(NT, 128, 32).transpose(1, 0, 2).copy()
    c["rope_s"] = Sg.reshape(NT, 128, 32).transpose(1, 0, 2).copy()
    return c


WEIGHT_SPECS = [
    ("norm_mix_gT", (128, 8)), ("norm_mlp_gT", (128, 8)), ("w_ada", (1024, 6144)), ("b_adaT", (128, 48)),
    ("w_in", (1024, DIN)), ("w_out", (1024, 1024)), ("sinks_b", (128, 4)), ("gqT", (128, 2)), ("gkvT", (128, 1)),
    ("w_uq", (192, 384)), ("w_ukv", (128, 512)), ("posT", (128, 32)), ("w1k", (128, 32, 128)), ("w1v", (128, 32, 128)),
    ("w2k", (128, 64)), ("w2v", (128, 64)), ("w_up", (1024, DFF)), ("w_down", (DFF, 1024)),
]


class Prog:
    def __init__(self, S, depth, dbg=(), stop_after=None, skip=None):
        self.S, self.depth, self.dbg, self.stop_after, self.skip = S, depth, set(dbg), stop_after, skip
        self.NT, self.NG = S // 128, S // 512
        nc = self.nc = bass.Bass("TRN2", target_bir_lowering=False)
        self.sc = Sched(nc)
        self.din = {}
        self.bufs = {}
        self._build()

    def inp(self, name, shape, dt=F32):
        t = self.nc.dram_tensor(name, list(shape), dt, kind="ExternalInput").ap()
        self.din[name] = t
        return t

    def scr(self, name, shape, dt):
        kind = "ExternalOutput" if name in self.dbg else "Internal"
        t = self.nc.dram_tensor(name, list(shape), dt, kind=kind).ap()
        self.bufs[name] = Buf(name)
        return t

    def sb(self, es, name, shape, dt):
        self._uid = getattr(self, "_uid", 0) + 1
        name = f"{name}_{self._uid}"
        t = es.enter_context(self.nc.sbuf_tensor(name, list(shape), dt))
        b = Buf(name)
        self.sc.op("pool", lambda e: e.memset(t[:], 0.0), [], [b])
        return t, b

    def cast_load(self, dst, src, dbuf, ncols):
        step = 2048
        for c0 in range(0, ncols, step):
            c1 = min(ncols, c0 + step)
            self.sc.dma("pool", dst[:, c0:c1], src[:, c0:c1], writes=[dbuf])

    def _build(self):
        nc, sc, S, NT, NG = self.nc, self.sc, self.S, self.NT, self.NG
        L = self.depth
        self.x_in = self.inp("x", (S, D))
        self.cT_in = self.inp("cT", (128, 8))
        self.fgb_in = self.inp("final_g", (1, D))
        import os
        self.light = bool(os.environ.get("KLIGHT"))
        self.w = {n: self.inp(n, (L,) + (((8, 8)) if (self.light and n in ("w_ada", "w_up", "w_down", "w_out")) else shp)) for n, shp in WEIGHT_SPECS}
        cshape = make_consts(S)
        self.cst = {n: self.inp("k_" + n, v.shape, BF16 if v.dtype == ml_dtypes.bfloat16 else F32) for n, v in cshape.items()}
        self.identf_in = self.inp("k_identf", (128, 128))
        self.out = self.nc.dram_tensor("out", [S, D], F32, kind="ExternalOutput").ap()
        self.bufs["out"] = Buf("out")
        self.xres = self.scr("xres", (S, D), F32)
        self.QT = {"A": self.scr("QT_A", (4, 69, S), BF16), "C": self.scr("QT_C", (4, 69, S), BF16),
                   "D": self.scr("QT_D", (4, 69, S), BF16), "B": self.scr("QT_B", (4, 96, S), BF16)}
        self.KT = {"A": self.scr("KT_A", (2, 69, S), BF16), "C": self.scr("KT_C", (2, 69, S), BF16),
                   "D": self.scr("KT_D", (4, 69, S), BF16), "B": self.scr("KT_B", (4, 96, S), BF16)}
        self.V = {"A": self.scr("V_A", (S, 2 * 65), BF16), "B": self.scr("V_B", (S, 4 * 65), BF16),
                  "C": self.scr("V_C", (S, 3 * 65), BF16), "D": self.scr("V_D", (S, 4 * 65), BF16)}
        self.kcvcT = self.scr("kcvcT", (128, S), F32)
        self.O = self.scr("O", (S, D), BF16)
        self.hT2 = self.scr("hT2", (8, 128, S), BF16)
        self.gsc = self.scr("gsc", (2, 8, 128), F32)
        with ExitStack() as es:
            self.banks = []
            for i in range(8):
                t = es.enter_context(nc.psum_tensor(f"bank{i}", [128, 512], F32))
                bb = Buf(f"bank{i}")
                sc.op("dve", lambda e: e.memset(t[:], 0.0), [], [bb])
                self.banks.append((t, bb))
            self.ident, b = self.sb(es, "ident", (128, 128), BF16)
            self.identf, b2 = self.sb(es, "identf", (128, 128), F32)
            sc.dma("sp", self.ident[:], self.cst["ident"], writes=[b])
            sc.dma("sp", self.identf[:], self.identf_in, writes=[b2])
            self.ident_b, self.identf_b = b, b2
            self.gates, self.gates_b = self.sb(es, "gates", (128, NT, 12), F32)
            self.epsb, self.epsb_b = self.sb(es, "epsb", (128, 1), F32)
            sc.op("dve", lambda e: e.memset(self.epsb[:], EPS), [], [self.epsb_b])
            self.ada, self.ada_b = self.sb(es, "ada", (128, 4, 8), F32)
            self.gb, self.gb_b = self.sb(es, "gb", (128, 2, D), F32)
            for mi, m in enumerate(("A", "C", "D")):
                for h in range(4):
                    sc.dma("sp", self.QT[m][h, 64:69, :], self.cst["qrows"][4 * mi + h], writes=[self.bufs["QT_" + m]])
                for h in range(self.KT[m].shape[0]):
                    sc.dma("sp", self.KT[m][h, 64:69, :], self.cst["krows"], writes=[self.bufs["KT_" + m]])
            sc.barrier()
            for l in range(L):
                if self.light:
                    sc.op("dve", lambda e: e.memset(self.ada[:], 1.0), [], [self.ada_b])
                    sc.op("dve", lambda e: e.memset(self.gb[:], 1.0), [], [self.gb_b])
                else:
                    self.phase0(l)
                sc.barrier()
                if self.stop_after == ("0", l):
                    break
                self.phaseA(l)
                sc.barrier()
                if self.stop_after == ("A", l):
                    break
                for nm, fn in (("N", lambda: self.phase_nsa(l)), ("S", lambda: self.phase_swa(l)), ("B", lambda: self.phase_full(l, "B")),
                               ("M", lambda: self.phase_full(l, "D")), ("D1", lambda: self.phase_D1(l)),
                               ("D2a", lambda: self.phase_D2(l, 0, False)), ("D2b", lambda: self.phase_D2(l, 1, l == L - 1))):
                    if self.skip and nm in self.skip:
                        continue
                    fn()
                    sc.barrier()
                    if self.stop_after == (nm, l):
                        break
                if self.stop_after is not None and self.stop_after[1] == l:
                    break
            sc.barrier()
            sc.final_wait([self.bufs[n] for n in self.bufs])

    def phase0(self, l):
        nc, sc = self.nc, self.sc
        with ExitStack() as es:
            cT, cT_b = self.sb(es, "p0_cT", (128, 8), F32)
            sg, sg_b = self.sb(es, "p0_sg", (128, 8), F32)
            s2, s2_b = self.sb(es, "p0_s2", (128, 8, 2), F32)
            bT, bT_b = self.sb(es, "p0_bT", (128, 48), F32)
            gm, gm_b = self.sb(es, "p0_gm", (128, 2, 8), F32)
            modT, modT_b = self.sb(es, "p0_modT", (128, 48), F32)
            gT, gT_b = self.sb(es, "p0_gT", (8, 128), F32)
            gTT, gTT_b = self.sb(es, "p0_gT2", (64, 128), F32)
            gT2 = gTT[32:40]
            wch = [self.sb(es, f"p0_w{i}", (128, 8, 512), F32) for i in range(2)]
            sc.dma("sp", cT[:], self.cT_in, writes=[cT_b])
            sc.dma("sp", bT[:], self.w["b_adaT"][l], writes=[bT_b])
            sc.dma("sp", gm[:, 0, :], self.w["norm_mix_gT"][l], writes=[gm_b])
            sc.dma("sp", gm[:, 1, :], self.w["norm_mlp_gT"][l], writes=[gm_b])
            sc.op("act", lambda e: e.activation(out=sg[:], in_=cT[:], func=AF.Sigmoid), [cT_b], [sg_b])
            sc.op("dve", lambda e: e.tensor_tensor(out=s2[:, :, 0], in0=cT[:], in1=sg[:], op=ALU.mult), [cT_b, sg_b], [s2_b])
            sc.op("dve", lambda e: e.tensor_tensor(out=s2[:, :, 1], in0=cT[:], in1=sg[:], op=ALU.mult), [cT_b, sg_b], [s2_b])
            bank, bank_b = self.banks[0]
            wsrc = self.w["w_ada"][l].rearrange("(c p) n -> p c n", p=128)
            for i in range(12):
                wt, wt_b = wch[i % 2]
                sc.dma("sp", wt[:], wsrc[:, :, 512 * i:512 * (i + 1)], writes=[wt_b])
                for s in range(4):
                    m = 4 * i + s
                    for c in range(8):
                        sc.op("pe", lambda e, c=c, s=s, m=m, wt=wt: e.matmul(
                            bank[:, 2 * m:2 * m + 2], wt[:, c, 128 * s:128 * (s + 1)], s2[:, c, :],
                            start=(c == 0), stop=(c == 7)), [wt_b, s2_b], [bank_b])
            sc.op("dve", lambda e: e.tensor_tensor(out=modT[:], in0=bank[:, 0:96:2], in1=bT[:], op=ALU.add),
                  [bank_b, bT_b], [modT_b])
            ada = self.ada
            sc.op("dve", lambda e: e.scalar_tensor_tensor(out=ada[:, 0, :], in0=modT[:, 8:16], scalar=1.0, in1=gm[:, 0, :],
                                                          op0=ALU.add, op1=ALU.mult), [modT_b, gm_b], [self.ada_b])
            sc.op("dve", lambda e: e.tensor_copy(out=ada[:, 1, :], in_=modT[:, 0:8]), [modT_b], [self.ada_b])
            sc.op("dve", lambda e: e.scalar_tensor_tensor(out=ada[:, 2, :], in0=modT[:, 32:40], scalar=1.0, in1=gm[:, 1, :],
                                                          op0=ALU.add, op1=ALU.mult), [modT_b, gm_b], [self.ada_b])
            sc.op("dve", lambda e: e.tensor_copy(out=ada[:, 3, :], in_=modT[:, 24:32]), [modT_b], [self.ada_b])
            bank1, bank1_b = self.banks[1]
            gg, gg_b = self.sb(es, "p0_gg", (128, 128), F32)
            sc.op("dve", lambda e: e.tensor_copy(out=gg[:, 0:8], in_=modT[:, 16:24]), [modT_b], [gg_b])
            sc.op("dve", lambda e: e.tensor_copy(out=gg[:, 32:40], in_=modT[:, 40:48]), [modT_b], [gg_b])
            sc.op("pe", lambda e: e.transpose(bank1[:, 0:128], gg[:], self.identf[:]), [gg_b, self.identf_b], [bank1_b])
            sc.op("act", lambda e: e.activation(out=gT[:, 0:128], in_=bank1[0:8, 0:128], func=AF.Copy), [bank1_b], [gT_b])
            sc.op("act", lambda e: e.activation(out=gT2[:, 0:128], in_=bank1[32:40, 0:128], func=AF.Copy), [bank1_b], [gTT_b])
            gs_b = self.bufs["gsc"]
            sc.dma("sp", self.gsc[0], gT[:, :], reads=[gT_b], writes=[gs_b])
            sc.dma("sp", self.gsc[1], gT2[:, :], reads=[gTT_b], writes=[gs_b])
            for k in range(2):
                src = self.gsc[k].rearrange("c p -> (c p)").rearrange("(o n) -> o n", o=1)
                sc.dma("sp", self.gb[:, k, :], src.broadcast_to([128, D]), reads=[gs_b], writes=[self.gb_b])

    def norm_to_hT(self, xt, xt_b, hT, hT_b, which, tmp):
        sc = self.sc
        ss, ss_b, rs, rs_b, xn, xn_b, junk, junk_b = tmp
        for j in range(4):
            sc.op("act", lambda e, j=j: e.activation(out=junk[:], in_=xt[:, j, :], func=AF.Square, accum_out=ss[:, j:j + 1]),
                  [xt_b], [junk_b, ss_b])
        sc.op("act", lambda e: e.activation(out=rs[:], in_=ss[:], func=AF.Sqrt, scale=1.0 / D, bias=self.epsb[:, 0:1]),
              [ss_b, self.epsb_b], [rs_b])
        sc.op("dve", lambda e: e.reciprocal(out=rs[:], in_=rs[:]), [rs_b], [rs_b])
        for j in range(4):
            sc.op("act", lambda e, j=j: e.activation(out=xn[:, j, :], in_=xt[:, j, :], func=AF.Copy, scale=rs[:, j:j + 1]),
                  [xt_b, rs_b], [xn_b])
        a_i, b_i = 2 * which, 2 * which + 1
        for c in range(8):
            bank, bank_b = self.banks[c % 2]
            pv = bank.bitcast(BF16)
            for j in range(4):
                sc.op("pe", lambda e, c=c, j=j, pv=pv: e.transpose(pv[:, 128 * j:128 * (j + 1)], xn[:, j, 128 * c:128 * (c + 1)], self.ident[:]),
                      [xn_b, self.ident_b], [bank_b])
            sc.op("dve", lambda e, c=c, pv=pv: e.tensor_scalar(out=hT[:, c, :], in0=pv[:, 0:512], scalar1=self.ada[:, a_i, c:c + 1],
                                                               scalar2=self.ada[:, b_i, c:c + 1], op0=ALU.mult, op1=ALU.add),
                  [bank_b, self.ada_b], [hT_b])

    def phaseA(self, l):
        nc, sc, S, NT, NG = self.nc, self.sc, self.S, self.NT, self.NG
        xsrc = self.x_in if l == 0 else self.xres
        xsrc_b = [] if l == 0 else [self.bufs["xres"]]
        with ExitStack() as es:
            win, win_b = self.sb(es, "A_win", (128, 8, DIN), BF16)
            wsrc = self.w["w_in"][l].rearrange("(c p) n -> p c n", p=128)
            for c in range(8):
                self.cast_load(win[:, c, :], wsrc[:, c, :], win_b, DIN)
            wuqf, wuqf_b = self.sb(es, "A_wuqf", (128, 2, 384), F32)
            wukvf, wukvf_b = self.sb(es, "A_wukvf", (128, 512), F32)
            wuq, wuq_b = self.sb(es, "A_wuq", (128, 2, 384), BF16)
            wukv, wukv_b = self.sb(es, "A_wukv", (128, 512), BF16)
            gq, gq_b = self.sb(es, "A_gq", (128, 3), F32)
            sc.dma("sp", wuqf[:, 0, :], self.w["w_uq"][l, 0:128, :], writes=[wuqf_b])
            sc.dma("sp", wuqf[0:64, 1, :], self.w["w_uq"][l, 128:192, :], writes=[wuqf_b])
            sc.dma("sp", wukvf[:], self.w["w_ukv"][l], writes=[wukvf_b])
            sc.dma("sp", gq[:, 0:2], self.w["gqT"][l], writes=[gq_b])
            sc.dma("sp", gq[:, 2:3], self.w["gkvT"][l], writes=[gq_b])
            sc.op("dve", lambda e: e.tensor_scalar(out=wuq[:, 0, :], in0=wuqf[:, 0, :], scalar1=gq[:, 0:1], scalar2=None, op0=ALU.mult),
                  [wuqf_b, gq_b], [wuq_b])
            sc.op("dve", lambda e: e.tensor_scalar(out=wuq[0:64, 1, :], in0=wuqf[0:64, 1, :], scalar1=gq[0:64, 1:2], scalar2=None, op0=ALU.mult),
                  [wuqf_b, gq_b], [wuq_b])
            sc.op("dve", lambda e: e.memset(wuq[64:128, 1, :], 0.0), [], [wuq_b])
            sc.op("dve", lambda e: e.tensor_scalar(out=wukv[:], in0=wukvf[:], scalar1=gq[:, 2:3], scalar2=None, op0=ALU.mult),
                  [wukvf_b, gq_b], [wukv_b])
            rc, rc_b = self.sb(es, "A_rc", (128, NT, 32), F32)
            rs_, rs__b = self.sb(es, "A_rs", (128, NT, 32), F32)
            sc.dma("sp", rc[:], self.cst["rope_c"], writes=[rc_b])
            sc.dma("sp", rs_[:], self.cst["rope_s"], writes=[rs__b])
            xt, xt_b = self.sb(es, "A_xt", (128, 4, D), F32)
            hT, hT_b = self.sb(es, "A_hT", (128, 8, 512), BF16)
            tmp = (*self.sb(es, "A_ss", (128, 4), F32), *self.sb(es, "A_rsd", (128, 4), F32),
                   *self.sb(es, "A_xn", (128, 4, D), BF16), *self.sb(es, "A_junk", (128, D), BF16))
            junk, junk_b = tmp[6], tmp[7]
            stg = [self.sb(es, f"A_stg{i}", (128, 512), BF16) for i in range(2)]
            stgf, stgf_b = self.sb(es, "A_stgf", (128, 512), F32)
            vA, vA_b = self.sb(es, "A_vA", (128, 4, 2, 65), BF16)
            vB, vB_b = self.sb(es, "A_vB", (128, 4, 4, 65), BF16)
            vC, vC_b = self.sb(es, "A_vC", (128, 4, 3, 65), BF16)
            vD, vD_b = self.sb(es, "A_vD", (128, 4, 4, 65), BF16)
            for vt, vb in ((vA, vA_b), (vB, vB_b), (vC, vC_b), (vD, vD_b)):
                sc.op("dve", lambda e, vt=vt: e.memset(vt[:], 1.0), [], [vb])
            st2, st2_b = self.sb(es, "A_st2", (128, 4), F32)
            cqn, cqn_b = self.sb(es, "A_cqn", (128, 320), BF16)
            cqnT, cqnT_b = self.sb(es, "A_cqnT", (128, 3, 128), BF16)
            sc.op("dve", lambda e: e.memset(cqnT[64:128, 1, :], 0.0), [], [cqnT_b])
            qr, qr_b = self.sb(es, "A_qr", (128, 4, 32), F32)
            rt1, rt1_b = self.sb(es, "A_rt1", (128, 4, 32), F32)
            rt2, rt2_b = self.sb(es, "A_rt2", (128, 4, 32), F32)
            kpr, kpr_b = self.sb(es, "A_kpr", (128, 32), F32)
            kt1, kt1_b = self.sb(es, "A_kt1", (128, 32), F32)
            kt2, kt2_b = self.sb(es, "A_kt2", (128, 32), F32)
            QB, QB_b = self.sb(es, "A_QB", (128, 4, 96), BF16)
            KB, KB_b = self.sb(es, "A_KB", (128, 4, 96), BF16)
            qTB, qTB_b = self.sb(es, "A_qTB", (96, 4, 512), BF16)
            kTB, kTB_b = self.sb(es, "A_kTB", (96, 4, 512), BF16)

            fm_specs = [
                (0, 128, "QT_A", 0, 0.125), (128, 128, "QT_A", 2, 0.125), (256, 128, "KT_A", 0, 1.0),
                (864, 128, "QT_C", 0, 0.125), (992, 128, "QT_C", 2, 0.125), (1120, 128, "kcvc", 0, 1.0),
                (1248, 128, "KT_C1", 0, 1.0), (1376, 128, "KT_C1", 1, 1.0),
                (1516, 128, "QT_D", 0, 0.125), (1644, 128, "QT_D", 2, 0.125), (1772, 128, "KT_D", 0, 1.0), (1900, 128, "KT_D", 2, 1.0),
            ]
            dst_of = {"QT_A": self.QT["A"], "QT_C": self.QT["C"], "QT_D": self.QT["D"],
                      "KT_A": self.KT["A"], "KT_C": self.KT["C"], "KT_D": self.KT["D"]}
            import os
            sub = int(os.environ.get("KSUB", "99"))
            for g in range(NG):
                t0 = 512 * g
                if sub < 1:
                    break
                sc.dma("sp", xt[:], xsrc[t0:t0 + 512, :].rearrange("(j p) f -> p j f", p=128), reads=xsrc_b, writes=[xt_b])
                if sub < 2:
                    break
                self.norm_to_hT(xt, xt_b, hT, hT_b, 0, tmp)
                if sub < 3:
                    break
                for fi, (c0, M, kind, idx, scl) in enumerate(fm_specs):
                    bank, bank_b = self.banks[2 + fi % 2]
                    for c in range(8):
                        sc.op("pe", lambda e, c=c, c0=c0, M=M, bank=bank: e.matmul(bank[0:M, :], win[:, c, c0:c0 + M], hT[:, c, :],
                                                                                 start=(c == 0), stop=(c == 7)), [win_b, hT_b], [bank_b])
                    if kind == "kcvc":
                        sc.op("act", lambda e, bank=bank: e.activation(out=stgf[:], in_=bank[:], func=AF.Copy), [bank_b], [stgf_b])
                        sc.dma("sp", self.kcvcT[:, t0:t0 + 512], stgf[:], reads=[stgf_b], writes=[self.bufs["kcvcT"]])
                        continue
                    sg_, sg_b = stg[fi % 2]
                    sc.op("act", lambda e, bank=bank, M=M, scl=scl, sg_=sg_: e.activation(out=sg_[0:M, :], in_=bank[0:M, :], func=AF.Copy, scale=scl),
                          [bank_b], [sg_b])
                    if kind == "KT_C1":
                        sc.dma("sp", self.KT["C"][idx, 0:64, t0:t0 + 512], sg_[0:64, :], reads=[sg_b], writes=[self.bufs["KT_C"]])
                        continue
                    for hh in range(M // 64):
                        sc.dma("sp", dst_of[kind][idx + hh, 0:64, t0:t0 + 512], sg_[64 * hh:64 * hh + 64, :], reads=[sg_b], writes=[self.bufs[kind]])
                for j in range(4 if sub >= 4 else 0):
                    n = 4 * g + j
                    b1, b1_b = self.banks[4]
                    b2, b2_b = self.banks[5]
                    for (bank, bank_b, o0, c0, w_) in ((b1, b1_b, 0, 384, 480), (b2, b2_b, 0, 1312, 204), (b2, b2_b, 204, 2028, 256)):
                        for c in range(8):
                            sc.op("pe", lambda e, c=c, bank=bank, o0=o0, c0=c0, w_=w_: e.matmul(
                                bank[:, o0:o0 + w_], hT[:, c, 128 * j:128 * (j + 1)], win[:, c, c0:c0 + w_], start=(c == 0), stop=(c == 7)),
                                [win_b, hT_b], [bank_b])
                    sc.op("act", lambda e: e.activation(out=vA[:, j, :, 0:64], in_=b1[:, 0:128].rearrange("p (h d) -> p h d", h=2), func=AF.Copy), [b1_b], [vA_b])
                    sc.op("act", lambda e: e.activation(out=vC[:, j, :, 0:64], in_=b2[:, 0:192].rearrange("p (h d) -> p h d", h=3), func=AF.Copy), [b2_b], [vC_b])
                    sc.op("act", lambda e: e.activation(out=self.gates[:, n, :], in_=b2[:, 192:204], func=AF.Sigmoid), [b2_b], [self.gates_b])
                    sc.op("act", lambda e: e.activation(out=vD[:, j, :, 0:64], in_=b2[:, 204:460].rearrange("p (h d) -> p h d", h=4), func=AF.Copy), [b2_b], [vD_b])
                    if sub < 5:
                        continue
                    sc.op("act", lambda e: e.activation(out=junk[:, 0:192], in_=b1[:, 128:320], func=AF.Square, accum_out=st2[:, 0:1]),
                          [b1_b], [junk_b, st2_b])
                    sc.op("act", lambda e: e.activation(out=junk[:, 0:128], in_=b1[:, 320:448], func=AF.Square, accum_out=st2[:, 1:2]),
                          [b1_b], [junk_b, st2_b])
                    sc.op("act", lambda e: e.activation(out=st2[:, 2:3], in_=st2[:, 0:1], func=AF.Sqrt, scale=1.0 / 192, bias=self.epsb[:, 0:1]),
                          [st2_b, self.epsb_b], [st2_b])
                    sc.op("act", lambda e: e.activation(out=st2[:, 3:4], in_=st2[:, 1:2], func=AF.Sqrt, scale=1.0 / 128, bias=self.epsb[:, 0:1]),
                          [st2_b, self.epsb_b], [st2_b])
                    sc.op("dve", lambda e: e.reciprocal(out=st2[:, 2:4], in_=st2[:, 2:4]), [st2_b], [st2_b])
                    sc.op("act", lambda e: e.activation(out=cqn[:, 0:192], in_=b1[:, 128:320], func=AF.Copy, scale=st2[:, 2:3]), [b1_b, st2_b], [cqn_b])
                    sc.op("act", lambda e: e.activation(out=cqn[:, 192:320], in_=b1[:, 320:448], func=AF.Copy, scale=st2[:, 3:4]), [b1_b, st2_b], [cqn_b])
                    sc.op("act", lambda e: e.activation(out=kpr[:], in_=b1[:, 448:480], func=AF.Copy), [b1_b], [kpr_b])
                    sc.op("dve", lambda e: e.tensor_tensor(out=kt1[:], in0=kpr[:], in1=rc[:, n, :], op=ALU.mult), [kpr_b, rc_b], [kt1_b])
                    sc.op("dve", lambda e: e.tensor_tensor(out=kt2[:, 0:16], in0=kpr[:, 16:32], in1=rs_[:, n, 0:16], op=ALU.mult), [kpr_b, rs__b], [kt2_b])
                    sc.op("dve", lambda e: e.tensor_tensor(out=kt2[:, 16:32], in0=kpr[:, 0:16], in1=rs_[:, n, 16:32], op=ALU.mult), [kpr_b, rs__b], [kt2_b])
                    sc.op("dve", lambda e: e.tensor_tensor(out=kt1[:], in0=kt1[:], in1=kt2[:], op=ALU.add), [kt1_b, kt2_b], [kt1_b])
                    for h in range(4):
                        sc.op("dve", lambda e, h=h: e.tensor_copy(out=KB[:, h, 64:96], in_=kt1[:]), [kt1_b], [KB_b])
                    if sub < 6:
                        continue
                    if os.environ.get("KDBG"):
                        print("MARK6", sc.count)
                    tb, tb_b = self.banks[0]
                    tbv = tb.bitcast(BF16)
                    sc.op("pe", lambda e: e.transpose(tbv[:, 0:128], cqn[:, 0:128], self.ident[:]), [cqn_b, self.ident_b], [tb_b])
                    sc.op("pe", lambda e: e.transpose(tbv[:, 128:256], cqn[:, 128:256], self.ident[:]), [cqn_b, self.ident_b], [tb_b])
                    sc.op("pe", lambda e: e.transpose(tbv[:, 256:384], cqn[:, 192:320], self.ident[:]), [cqn_b, self.ident_b], [tb_b])
                    sc.op("dve", lambda e: e.tensor_copy(out=cqnT[:, 0, :], in_=tbv[:, 0:128]), [tb_b], [cqnT_b])
                    sc.op("dve", lambda e: e.tensor_copy(out=cqnT[:, 1, :], in_=tbv[:, 128:256]), [tb_b], [cqnT_b])
                    sc.op("dve", lambda e: e.tensor_copy(out=cqnT[:, 2, :], in_=tbv[:, 256:384]), [tb_b], [cqnT_b])
                    b6, b6_b = self.banks[2]
                    b7, b7_b = self.banks[3]
                    sc.op("pe", lambda e: e.matmul(b6[:, 0:384], cqnT[:, 0, :], wuq[:, 0, :], start=True, stop=False), [cqnT_b, wuq_b], [b6_b])
                    sc.op("pe", lambda e: e.matmul(b6[:, 0:384], cqnT[:, 1, :], wuq[:, 1, :], start=False, stop=True), [cqnT_b, wuq_b], [b6_b])
                    sc.op("pe", lambda e: e.matmul(b7[:, 0:512], cqnT[:, 2, :], wukv[:], start=True, stop=True), [cqnT_b, wukv_b], [b7_b])
                    q3 = b6[:, 0:384].rearrange("p (h d) -> p h d", h=4)
                    kv3 = b7[:, 0:512].rearrange("p (h d) -> p h d", h=4)
                    sc.op("act", lambda e: e.activation(out=QB[:, :, 0:64], in_=q3[:, :, 0:64], func=AF.Copy), [b6_b], [QB_b])
                    sc.op("act", lambda e: e.activation(out=KB[:, :, 0:64], in_=kv3[:, :, 0:64], func=AF.Copy), [b7_b], [KB_b])
                    sc.op("act", lambda e: e.activation(out=vB[:, j, :, 0:64], in_=kv3[:, :, 64:128], func=AF.Copy),
                          [b7_b], [vB_b])
                    if sub < 7:
                        continue
                    sc.op("act", lambda e: e.activation(out=qr[:], in_=q3[:, :, 64:96], func=AF.Copy), [b6_b], [qr_b])
                    for h in range(4):
                        sc.op("dve", lambda e: e.tensor_tensor(out=rt1[:, h, :], in0=qr[:, h, :], in1=rc[:, n, :], op=ALU.mult), [qr_b, rc_b], [rt1_b])
                        sc.op("dve", lambda e: e.tensor_tensor(out=rt2[:, h, 0:16], in0=qr[:, h, 16:32], in1=rs_[:, n, 0:16], op=ALU.mult), [qr_b, rs__b], [rt2_b])
                        sc.op("dve", lambda e: e.tensor_tensor(out=rt2[:, h, 16:32], in0=qr[:, h, 0:16], in1=rs_[:, n, 16:32], op=ALU.mult), [qr_b, rs__b], [rt2_b])
                    sc.op("dve", lambda e: e.tensor_tensor(out=QB[:, :, 64:96], in0=rt1[:], in1=rt2[:], op=ALU.add), [rt1_b, rt2_b], [QB_b])
                    if sub < 8:
                        continue
                    if os.environ.get("KDBG"):
                        print("MARK8", sc.count)
                    tq, tq_b = self.banks[1]
                    tqv = tq.bitcast(BF16)
                    for h in range(4):
                        sc.op("pe", lambda e, h=h: e.transpose(tqv[0:96, 128 * h:128 * (h + 1)], QB[:, h, :], self.ident[:]), [QB_b, self.ident_b], [tq_b])
                    for h in range(4):
                        sc.op("pe", lambda e, h=h: e.transpose(tqv[0:96, 512 + 128 * h:512 + 128 * (h + 1)], KB[:, h, :], self.ident[:]),
                              [KB_b, self.ident_b], [tq_b])
                    sc.op("act", lambda e: e.activation(out=qTB[:, :, 128 * j:128 * (j + 1)], in_=tqv[0:96, 0:512].rearrange("p (h t) -> p h t", h=4), func=AF.Copy),
                          [tq_b], [qTB_b])
                    sc.op("act", lambda e: e.activation(out=kTB[:, :, 128 * j:128 * (j + 1)], in_=tqv[0:96, 512:1024].rearrange("p (h t) -> p h t", h=4), func=AF.Copy),
                          [tq_b], [kTB_b])
                if os.environ.get("KDBG"):
                    print("MARKST", sc.count)
                sc.dma("sp", self.V["A"][t0:t0 + 512, :].rearrange("(j p) f -> p j f", p=128), vA[:].rearrange("p j h d -> p j (h d)"), reads=[vA_b], writes=[self.bufs["V_A"]])
                sc.dma("sp", self.V["B"][t0:t0 + 512, :].rearrange("(j p) f -> p j f", p=128), vB[:].rearrange("p j h d -> p j (h d)"), reads=[vB_b], writes=[self.bufs["V_B"]])
                sc.dma("sp", self.V["C"][t0:t0 + 512, :].rearrange("(j p) f -> p j f", p=128), vC[:].rearrange("p j h d -> p j (h d)"), reads=[vC_b], writes=[self.bufs["V_C"]])
                sc.dma("sp", self.V["D"][t0:t0 + 512, :].rearrange("(j p) f -> p j f", p=128), vD[:].rearrange("p j h d -> p j (h d)"), reads=[vD_b], writes=[self.bufs["V_D"]])
                sc.dma("sp", self.QT["B"][:, :, t0:t0 + 512].rearrange("h d t -> d h t"), qTB[:], reads=[qTB_b], writes=[self.bufs["QT_B"]])
                sc.dma("sp", self.KT["B"][:, :, t0:t0 + 512].rearrange("h d t -> d h t"), kTB[:], reads=[kTB_b], writes=[self.bufs["KT_B"]])


def host_weights(inp, L):
    f = lambda a: np.ascontiguousarray(np.asarray(a, np.float32))
    w = {}
    w["norm_mix_gT"] = f(np.asarray(inp["norm_mix_g"]).reshape(L, 8, 128).transpose(0, 2, 1))
    w["norm_mlp_gT"] = f(np.asarray(inp["norm_mlp_g"]).reshape(L, 8, 128).transpose(0, 2, 1))
    w["w_ada"] = f(inp["w_ada"])
    w["b_adaT"] = f(np.asarray(inp["b_ada"]).reshape(L, 48, 128).transpose(0, 2, 1))
    w["w_in"] = f(inp["w_in"])
    w["w_out"] = f(inp["w_out"])
    w["sinks_b"] = f(np.broadcast_to(np.asarray(inp["swa_sinks"])[:, None, :], (L, 128, 4)))
    gq = np.zeros((L, 256), np.float32)
    gq[:, :192] = np.asarray(inp["mla_q_norm_g"])
    w["gqT"] = f(gq.reshape(L, 2, 128).transpose(0, 2, 1))
    w["gkvT"] = f(np.asarray(inp["mla_kv_norm_g"]).reshape(L, 128, 1))
    w["w_uq"] = f(inp["mla_w_uq"])
    w["w_ukv"] = f(inp["mla_w_ukv"])
    pk = np.asarray(inp["nsa_cmp_pos_k"]).transpose(0, 2, 1)
    pv = np.asarray(inp["nsa_cmp_pos_v"]).transpose(0, 2, 1)
    w["posT"] = f(np.concatenate([pk, pv], axis=1))
    w1k = np.asarray(inp["nsa_cmp_k_w1"]).reshape(L, 32, 64, 128).transpose(0, 2, 1, 3)
    w1v = np.asarray(inp["nsa_cmp_v_w1"]).reshape(L, 32, 64, 128).transpose(0, 2, 1, 3)
    w["w1k"] = f(np.concatenate([w1k, np.zeros_like(w1k)], axis=1))
    w["w1v"] = f(np.concatenate([np.zeros_like(w1v), w1v], axis=1))
    w["w2k"] = f(inp["nsa_cmp_k_w2"])
    w["w2v"] = f(inp["nsa_cmp_v_w2"])
    w["w_up"] = f(inp["w_up"])
    w["w_down"] = f(inp["w_down"])
    return w


def core_inputs(inp, S, L, b, consts, hw):
    m = {"x": np.ascontiguousarray(np.asarray(inp["x"], np.float32)[b]),
         "cT": np.ascontiguousarray(np.asarray(inp["c"], np.float32)[b].reshape(8, 128).T),
         "final_g": np.ascontiguousarray(np.asarray(inp["final_norm_g"], np.float32).reshape(1, D)),
         "k_identf": np.eye(128, dtype=np.float32)}
    m.update(hw)
    for n, v in consts.items():
        m["k_" + n] = v
    return m


def _attn_group(self, QT, QT_b, ktiles, nblk, acc, acc_b, scale, pts):
    sc = self.sc
    ncol = nblk * 128
    pend = None
    first = True

    def pv(item, first):
        i, kt, pT, pT_b = item
        for b in range(nblk):
            sc.op("pe", lambda e, b=b: e.matmul(acc[:, 128 * b:128 * b + 65], pT[:, 128 * b:128 * (b + 1)], kt["v"][b],
                                              start=(first and b == 0), stop=True, skip_group_check=True),
                  [pT_b] + kt["vb"], [acc_b])
    for i, kt in enumerate(ktiles):
        bank, bank_b = self.banks[i % 3]
        ex = kt.get("extras", [])
        sc.op("pe", lambda e: e.matmul(bank[:, 0:ncol], kt["kT"], QT[:, 0:ncol], start=True, stop=True),
              [QT_b] + kt["kb"], [bank_b])
        for xi, (lh, rh, c0, nc_, xb) in enumerate(ex):
            sc.op("pe", lambda e: e.matmul(bank[:, c0:c0 + nc_], lh, rh, start=False, stop=True, skip_group_check=True),
                  xb, [bank_b])
        pT, pT_b = pts[i % 3]
        sc.op("act", lambda e: e.activation(out=pT[:, 0:ncol], in_=bank[:, 0:ncol], func=AF.Exp, scale=scale), [bank_b], [pT_b])
        if pend is not None:
            pv(pend, first)
            first = False
        pend = (i, kt, pT, pT_b)
    pv(pend, first)


def _finish(self, acc, acc_b, nblk, rd, rd_b, extra_den=None):
    sc = self.sc
    den = acc[:, 0:128 * nblk].rearrange("p (b d) -> p b d", d=128)[:, :, 64]
    sc.op("act", lambda e: e.activation(out=rd[:, 0:nblk], in_=den, func=AF.Copy), [acc_b], [rd_b])
    if extra_den is not None:
        ed, ed_b = extra_den
        sc.op("dve", lambda e: e.tensor_tensor(out=rd[:, 0:nblk], in0=rd[:, 0:nblk], in1=ed, op=ALU.add), [rd_b, ed_b], [rd_b])
    sc.op("dve", lambda e: e.tensor_scalar(out=rd[:, 0:nblk], in0=rd[:, 0:nblk], scalar1=1e-30, scalar2=None, op0=ALU.max), [rd_b], [rd_b])
    sc.op("dve", lambda e: e.reciprocal(out=rd[:, 0:nblk], in_=rd[:, 0:nblk]), [rd_b], [rd_b])


def _load_const(self, es, name, key, shape, dt):
    t, b = self.sb(es, name, shape, dt)
    self.sc.dma("sp", t[:], self.cst[key], writes=[b])
    return t, b


def phase_cmp(self, l, KcT, KcT_b, Vc, Vc_b):
    sc, S = self.sc, self.S
    ncmp = S // 16 - 1
    with ExitStack() as es:
        kv, kv_b = self.sb(es, "c_kv", (128, S), F32)
        sc.dma("sp", kv[:], self.kcvcT, reads=[self.bufs["kcvcT"]], writes=[kv_b])
        posT, posT_b = self.sb(es, "c_pos", (128, 32), F32)
        sc.dma("sp", posT[:], self.w["posT"][l], writes=[posT_b])
        w1k, w1k_b = self.sb(es, "c_w1k", (128, 32, 128), BF16)
        w1v, w1v_b = self.sb(es, "c_w1v", (128, 32, 128), BF16)
        for (wt, wb, nm) in ((w1k, w1k_b, "w1k"), (w1v, w1v_b, "w1v")):
            src = self.w[nm][l].rearrange("p l h -> p (l h)")
            self.cast_load(wt[:].rearrange("p l h -> p (l h)"), src, wb, 32 * 128)
        w2, w2_b = self.sb(es, "c_w2", (128, 2, 128), BF16)
        sc.dma("pool", w2[:, 0, 0:64], self.w["w2k"][l], writes=[w2_b])
        sc.dma("pool", w2[:, 1, 0:64], self.w["w2v"][l], writes=[w2_b])
        tmp, tmp_b = self.sb(es, "c_tmp", (128, 32, 512), BF16)
        sc.op("dve", lambda e: e.memset(tmp[:], 0.0), [], [tmp_b])
        for ll in range(32):
            sc.op("dve", lambda e: e.tensor_scalar(out=tmp[:, ll, 0:ncmp], in0=kv[:, ll:ll + 16 * (ncmp - 1) + 1:16], scalar1=posT[:, ll:ll + 1],
                                                   scalar2=None, op0=ALU.add), [kv_b, posT_b], [tmp_b])
        x2, x2_b = self.sb(es, "c_x2", (128, 512), F32)
        u, u_b = self.sb(es, "c_u", (128, 512), F32)
        gT = [self.sb(es, f"c_g{i}", (128, 512), BF16) for i in range(2)]
        for bi, (wt, wb) in enumerate(((w1k, w1k_b), (w1v, w1v_b))):
            bank, bank_b = self.banks[bi]
            for ll in range(32):
                sc.op("pe", lambda e: e.matmul(bank[:, :], wt[:, ll, :], tmp[:, ll, :], start=(ll == 0), stop=(ll == 31)), [wb, tmp_b], [bank_b])
            g_, g_b = gT[bi]
            sc.op("act", lambda e: e.activation(out=x2[:], in_=bank[:], func=AF.Square), [bank_b], [x2_b])
            sc.op("dve", lambda e: e.tensor_scalar(out=x2[:], in0=x2[:], scalar1=0.044715, scalar2=1.0, op0=ALU.mult, op1=ALU.add), [x2_b], [x2_b])
            sc.op("dve", lambda e: e.tensor_tensor(out=u[:], in0=bank[:], in1=x2[:], op=ALU.mult), [bank_b, x2_b], [u_b])
            sc.op("act", lambda e: e.activation(out=u[:], in_=u[:], func=AF.Sigmoid, scale=1.5957691216057308), [u_b], [u_b])
            sc.op("dve", lambda e: e.tensor_tensor(out=g_[:], in0=bank[:], in1=u[:], op=ALU.mult), [bank_b, u_b], [g_b])
        bank, bank_b = self.banks[2]
        sc.op("pe", lambda e: e.matmul(bank[:, :], w2[:, 0, :], gT[0][0][:], start=True, stop=True), [w2_b, gT[0][1]], [bank_b])
        sc.op("act", lambda e: e.activation(out=KcT[0:64, :], in_=bank[0:64, :], func=AF.Copy), [bank_b], [KcT_b])
        sc.dma("sp", KcT[64:69, :], self.cst["krows_c"], writes=[KcT_b])
        bank, bank_b = self.banks[3]
        for ct in range(4):
            sc.op("pe", lambda e: e.matmul(bank[:, 64 * ct:64 * ct + 64], gT[1][0][:, 128 * ct:128 * (ct + 1)], w2[:, 1, 0:64], start=True, stop=True),
                  [w2_b, gT[1][1]], [bank_b])
        sc.op("act", lambda e: e.activation(out=Vc[:, :, 0:64], in_=bank[:, 0:256].rearrange("p (c d) -> p c d", c=4), func=AF.Copy), [bank_b], [Vc_b])
        onc, onc_b = self.sb(es, "c_onc", (128, 4), BF16)
        sc.dma("sp", onc[:], self.cst["ones_c"], writes=[onc_b])
        sc.op("dve", lambda e: e.tensor_copy(out=Vc[:, :, 64], in_=onc[:]), [onc_b], [Vc_b])
    sc.barrier()


def _store_o(self, ot, ot_b, rows, col0, width):
    self.sc.dma("sp", self.O[rows[0]:rows[1], col0:col0 + width], ot, reads=[ot_b], writes=[self.bufs["O"]])


def phase_nsa(self, l):
    sc, S, NT = self.sc, self.S, self.NT
    nct = (S // 16 - 1 + 127) // 128
    with ExitStack() as es:
        KcT, KcT_b = self.sb(es, "n_KcT", (128, 512), BF16)
        Vc, Vc_b = self.sb(es, "n_Vc", (128, 4, 65), BF16)
        sc.op("dve", lambda e: e.memset(KcT[:], 0.0), [], [KcT_b])
        self.phase_cmp(l, KcT, KcT_b, Vc, Vc_b)
        KT, KT_b = self.sb(es, "n_KT", (128, 2, S), BF16)
        sc.op("dve", lambda e: e.memset(KT[:], 0.0), [], [KT_b])
        for i in range(2):
            sc.dma("sp", KT[0:69, i, :], self.KT["C"][i], reads=[self.bufs["KT_C"]], writes=[KT_b])
        Vt, Vt_b = self.sb(es, "n_V", (128, NT, 3 * 65), BF16)
        sc.dma("sp", Vt[:], self.V["C"].rearrange("(n p) f -> p n f", p=128), reads=[self.bufs["V_C"]], writes=[Vt_b])
        gc, gc_b = _load_const(self, es, "n_gc", "g_c", (128, S), BF16)
        mpool, mpool_b = _load_const(self, es, "n_mpool", "mpool", (128, 4, 128), BF16)
        mbc, mbc_b = _load_const(self, es, "n_mbc", "mb_cmp", (128, 17, 128), BF16)
        mbd, mbd_b = _load_const(self, es, "n_mbd", "mb_diag4", (128, 512), BF16)
        mbw, mbw_b = _load_const(self, es, "n_mbw", "mb_win4", (128, 512), BF16)
        tA, tA_b = _load_const(self, es, "n_tA", "nsa_A", (128, 254), F32)
        tB, tB_b = _load_const(self, es, "n_tB", "nsa_B", (128, 254), F32)
        QTs = [self.sb(es, f"n_QT{i}", (128, 512), BF16) for i in range(2)]
        for t, b in QTs:
            sc.op("dve", lambda e, t=t: e.memset(t[:], 0.0), [], [b])
        pts = [self.sb(es, f"n_pT{i}", (128, 512), BF16) for i in range(3)]
        cpts = [self.sb(es, f"n_cpT{i}", (128, 512), BF16) for i in range(4)]
        rd, rd_b = self.sb(es, "n_rd", (128, 4), F32)
        mg, mg_b = self.sb(es, "n_mg", (128, 4), F32)
        imp, imp_b = self.sb(es, "n_imp", (128, 128), F32)
        imq, imq_b = self.sb(es, "n_imq", (128, 128), F32)
        m8, m8_b = self.sb(es, "n_m8", (128, 16), F32)
        selb, selb_b = self.sb(es, "n_selb", (128, 128), BF16)
        selT, selT_b = self.sb(es, "n_selT", (128, 512), BF16)
        oacc, oacc_b = self.sb(es, "n_oacc", (128, 4, 64), F32)
        ot, ot_b = self.sb(es, "n_ot", (128, 256), BF16)
        b5, b5_b = self.banks[5]
        for n in range(NT):
            QT, QT_b = QTs[n % 2]
            sc.dma("sp", QT[0:69, :].rearrange("p (h t) -> p h t", h=4), self.QT["C"][:, :, 128 * n:128 * (n + 1)].rearrange("h d t -> d h t"),
                   reads=[self.bufs["QT_C"]], writes=[QT_b])
            acc, acc_b = self.banks[3]
            kts = []
            nct_n = min(nct - 1, (8 * n + 6) // 128) + 1
            for ct in range(nct_n):
                dl = n - 16 * ct
                ex = []
                if dl <= 16:
                    for h in range(4):
                        ex.append((self.ident[:], mbc[:, dl, :], 128 * h, 128, [self.ident_b, mbc_b]))
                kts.append(dict(kT=KcT[:, 128 * ct:128 * (ct + 1)], kb=[KcT_b], extras=ex, v=[Vc[:, ct, :]] * 4, vb=[Vc_b]))
            _attn_group_keep(self, QT, QT_b, kts, acc, acc_b, cpts, mpool, mpool_b, b5, b5_b)
            _finish(self, acc, acc_b, 4, rd, rd_b)
            i3 = b5[:, :].rearrange("p (h j) -> p h j", h=4)
            sc.op("dve", lambda e: e.tensor_scalar(out=imp[:], in0=i3[:, 0, :], scalar1=rd[:, 0:1], scalar2=None, op0=ALU.mult), [b5_b, rd_b], [imp_b])
            for h in range(1, 4):
                sc.op("dve", lambda e: e.scalar_tensor_tensor(out=imp[:], in0=i3[:, h, :], scalar=rd[:, h:h + 1], in1=imp[:], op0=ALU.mult, op1=ALU.add),
                      [b5_b, rd_b, imp_b], [imp_b])
            sc.op("dve", lambda e: e.tensor_tensor(out=mg[:], in0=rd[:], in1=self.gates[:, n, 0:4], op=ALU.mult), [rd_b, self.gates_b], [mg_b])
            for h in range(4):
                sc.op("dve", lambda e: e.tensor_scalar(out=oacc[:, h, :], in0=acc[:, 128 * h:128 * h + 64], scalar1=mg[:, h:h + 1], scalar2=None, op0=ALU.mult),
                      [acc_b, mg_b], [oacc_b])
            o0 = 126 - 2 * n
            if o0 >= 0:
                tAs, tBs = tA[:, o0:o0 + 128], tB[:, o0:o0 + 128]
                sc.op("dve", lambda e: e.tensor_tensor(out=imq[:], in0=imp[:], in1=tAs, op=ALU.mult), [imp_b, tA_b], [imq_b])
                sc.op("dve", lambda e: e.tensor_tensor(out=imq[:], in0=imq[:], in1=tBs, op=ALU.add), [imq_b, tB_b], [imq_b])
            else:
                raise NotImplementedError
            sc.op("dve", lambda e: e.memset(imq[:, 0:1], BIGV), [], [imq_b])
            sc.op("dve", lambda e: e.max(out=m8[:, 0:8], in_=imq[:]), [imq_b], [m8_b])
            sc.op("dve", lambda e: e.match_replace(out=imp[:], in_to_replace=m8[:, 0:8], in_values=imq[:], imm_value=-3.0e38), [imq_b, m8_b], [imp_b])
            sc.op("dve", lambda e: e.max(out=m8[:, 8:16], in_=imp[:]), [imp_b], [m8_b])
            sc.op("dve", lambda e: e.tensor_scalar(out=imq[:], in0=imq[:], scalar1=m8[:, 15:16], scalar2=None, op0=ALU.is_ge), [imq_b, m8_b], [imq_b])
            sc.op("dve", lambda e: e.tensor_scalar(out=selb[:], in0=imq[:], scalar1=-1.0, scalar2=-NEGB, op0=ALU.add, op1=ALU.mult), [imq_b], [selb_b])
            sv = b5.bitcast(BF16)
            sc.op("pe", lambda e: e.transpose(sv[:, 0:128], selb[:], self.ident[:]), [selb_b, self.ident_b], [b5_b])
            for h in range(4):
                sc.op("act", lambda e: e.activation(out=selT[:, 128 * h:128 * (h + 1)], in_=sv[:, 0:128], func=AF.Copy), [b5_b], [selT_b])
            acc2, acc2_b = self.banks[4]
            kts = []
            for kt in range(n + 1):
                ex = [(gc[:, 128 * kt:128 * (kt + 1)], selT[:], 0, 512, [gc_b, selT_b])]
                if kt == n:
                    ex.append((self.ident[:], mbd[:], 0, 512, [self.ident_b, mbd_b]))
                kts.append(dict(kT=KT[:, 0, 128 * kt:128 * (kt + 1)], kb=[KT_b], extras=ex, v=[Vt[:, kt, 0:65]] * 4, vb=[Vt_b]))
            _attn_group(self, QT, QT_b, kts, 4, acc2, acc2_b, 1.0, pts)
            _finish(self, acc2, acc2_b, 4, rd, rd_b)
            sc.op("dve", lambda e: e.tensor_tensor(out=mg[:], in0=rd[:], in1=self.gates[:, n, 4:8], op=ALU.mult), [rd_b, self.gates_b], [mg_b])
            for h in range(4):
                sc.op("dve", lambda e: e.scalar_tensor_tensor(out=oacc[:, h, :], in0=acc2[:, 128 * h:128 * h + 64], scalar=mg[:, h:h + 1], in1=oacc[:, h, :],
                                                              op0=ALU.mult, op1=ALU.add), [acc2_b, mg_b, oacc_b], [oacc_b])
            kts = []
            for kt in range(max(0, n - 4), n + 1):
                ex = []
                if kt == n:
                    ex.append((self.ident[:], mbd[:], 0, 512, [self.ident_b, mbd_b]))
                if kt == n - 4:
                    ex.append((self.ident[:], mbw[:], 0, 512, [self.ident_b, mbw_b]))
                kts.append(dict(kT=KT[:, 1, 128 * kt:128 * (kt + 1)], kb=[KT_b], extras=ex, v=[Vt[:, kt, 130:195]] * 4, vb=[Vt_b]))
            _attn_group(self, QT, QT_b, kts, 4, acc, acc_b, 1.0, pts)
            _finish(self, acc, acc_b, 4, rd, rd_b)
            sc.op("dve", lambda e: e.tensor_tensor(out=mg[:], in0=rd[:], in1=self.gates[:, n, 8:12], op=ALU.mult), [rd_b, self.gates_b], [mg_b])
            for h in range(4):
                sc.op("dve", lambda e: e.scalar_tensor_tensor(out=ot[:, 64 * h:64 * h + 64], in0=acc[:, 128 * h:128 * h + 64], scalar=mg[:, h:h + 1], in1=oacc[:, h, :],
                                                              op0=ALU.mult, op1=ALU.add), [acc_b, mg_b, oacc_b], [ot_b])
            _store_o(self, ot[:], ot_b, (128 * n, 128 * n + 128), 512, 256)


def _attn_group_keep(self, QT, QT_b, ktiles, acc, acc_b, cpts, mpool, mpool_b, ib, ib_b):
    sc = self.sc
    for i, kt in enumerate(ktiles):
        bank, bank_b = self.banks[i % 3]
        ex = kt.get("extras", [])
        sc.op("pe", lambda e: e.matmul(bank[:, 0:512], kt["kT"], QT[:, 0:512], start=True, stop=True), [QT_b] + kt["kb"], [bank_b])
        for xi, (lh, rh, c0, nc_, xb) in enumerate(ex):
            sc.op("pe", lambda e: e.matmul(bank[:, c0:c0 + nc_], lh, rh, start=False, stop=True, skip_group_check=True), xb, [bank_b])
        pT, pT_b = cpts[i]
        sc.op("act", lambda e: e.activation(out=pT[:], in_=bank[:], func=AF.Exp), [bank_b], [pT_b])
    for i, kt in enumerate(ktiles):
        pT, pT_b = cpts[i]
        for b in range(4):
            sc.op("pe", lambda e: e.matmul(acc[:, 128 * b:128 * b + 65], pT[:, 128 * b:128 * (b + 1)], kt["v"][b], start=(i == 0 and b == 0), stop=True,
                                           skip_group_check=True), [pT_b] + kt["vb"], [acc_b])
    for i, kt in enumerate(ktiles):
        pT, pT_b = cpts[i]
        for b in range(4):
            sc.op("pe", lambda e: e.matmul(ib[:, 128 * b:128 * (b + 1)], pT[:, 128 * b:128 * (b + 1)], mpool[:, i, :], start=(i == 0 and b == 0), stop=True,
                                           skip_group_check=True), [pT_b, mpool_b], [ib_b])


def phase_swa(self, l):
    sc, S, NT = self.sc, self.S, self.NT
    with ExitStack() as es:
        KT, KT_b = self.sb(es, "s_KT", (128, 2, S), BF16)
        sc.op("dve", lambda e: e.memset(KT[:], 0.0), [], [KT_b])
        for i in range(2):
            sc.dma("sp", KT[0:69, i, :], self.KT["A"][i], reads=[self.bufs["KT_A"]], writes=[KT_b])
        Vt, Vt_b = self.sb(es, "s_V", (128, NT, 2 * 65), BF16)
        sc.dma("sp", Vt[:], self.V["A"].rearrange("(n p) f -> p n f", p=128), reads=[self.bufs["V_A"]], writes=[Vt_b])
        mbd, mbd_b = _load_const(self, es, "s_mbd", "mb_diag4", (128, 512), BF16)
        mbw, mbw_b = _load_const(self, es, "s_mbw", "mb_win4", (128, 512), BF16)
        esk, esk_b = self.sb(es, "s_esk", (128, 4), F32)
        sc.dma("sp", esk[:], self.w["sinks_b"][l], writes=[esk_b])
        sc.op("act", lambda e: e.activation(out=esk[:], in_=esk[:], func=AF.Exp), [esk_b], [esk_b])
        QTs = [self.sb(es, f"s_QT{i}", (128, 512), BF16) for i in range(2)]
        for t, b in QTs:
            sc.op("dve", lambda e, t=t: e.memset(t[:], 0.0), [], [b])
        pts = [self.sb(es, f"s_pT{i}", (128, 512), BF16) for i in range(3)]
        rd, rd_b = self.sb(es, "s_rd", (128, 4), F32)
        ots = [self.sb(es, f"s_ot{i}", (128, 256), BF16) for i in range(2)]
        for n in range(NT):
            QT, QT_b = QTs[n % 2]
            ot, ot_b = ots[n % 2]
            sc.dma("sp", QT[0:69, :].rearrange("p (h t) -> p h t", h=4), self.QT["A"][:, :, 128 * n:128 * (n + 1)].rearrange("h d t -> d h t"),
                   reads=[self.bufs["QT_A"]], writes=[QT_b])
            for kv in range(2):
                acc, acc_b = self.banks[3 + kv]
                kts = []
                for kt in range(max(0, n - 1), n + 1):
                    ex = [(self.ident[:], (mbd if kt == n else mbw)[:, 0:256], 0, 256, [self.ident_b, mbd_b, mbw_b])]
                    kts.append(dict(kT=KT[:, kv, 128 * kt:128 * (kt + 1)], kb=[KT_b], extras=ex, v=[Vt[:, kt, 65 * kv:65 * kv + 65]] * 2, vb=[Vt_b]))
                _attn_group(self, QT[:, 256 * kv:256 * kv + 256], QT_b, kts, 2, acc, acc_b, 1.0, pts)
                _finish(self, acc, acc_b, 2, rd, rd_b, extra_den=(esk[:, 2 * kv:2 * kv + 2], esk_b))
                for i in range(2):
                    h = 2 * kv + i
                    sc.op("dve", lambda e: e.tensor_scalar(out=ot[:, 64 * h:64 * h + 64], in0=acc[:, 128 * i:128 * i + 64], scalar1=rd[:, i:i + 1], scalar2=None,
                                                           op0=ALU.mult), [acc_b, rd_b], [ot_b])
            _store_o(self, ot[:], ot_b, (128 * n, 128 * n + 128), 0, 256)


def phase_full(self, l, mix):
    sc, S, NT, NG = self.sc, self.S, self.NT, self.NG
    kd = 96 if mix == "B" else 69
    scale = (96.0 ** -0.5) if mix == "B" else 1.0
    col0 = 256 if mix == "B" else 768
    nblk = S // 256
    with ExitStack() as es:
        KTs = [self.sb(es, f"f_KT{i}", (128, S), BF16) for i in range(2)]
        Vts = [self.sb(es, f"f_V{i}", (128, NT, 65), BF16) for i in range(2)]
        for t, b in KTs:
            sc.op("dve", lambda e, t=t: e.memset(t[:], 0.0), [], [b])
        mbg, mbg_b = _load_const(self, es, "f_mbg", "mb_grp", (128, 4, 512), BF16)
        QTs = [self.sb(es, f"f_QT{i}", (128, 512), BF16) for i in range(2)]
        for t, b in QTs:
            sc.op("dve", lambda e, t=t: e.memset(t[:], 0.0), [], [b])
        pts = [self.sb(es, f"f_pT{i}", (128, 512), BF16) for i in range(3)]
        rd, rd_b = self.sb(es, "f_rd", (128, 4), F32)
        ots = [self.sb(es, f"f_ot{i}", (128, 4, 64), BF16) for i in range(2)]
        if mix == "D":
            gd, gd_b = _load_const(self, es, "f_gd", "g_d", (128, S), BF16)
            mm, mm_b = _load_const(self, es, "f_mm", "moba_m", (128, 64), F32)
            kmf, kmf_b = self.sb(es, "f_kmf", (128, 32), F32)
            km, km_b = self.sb(es, "f_km", (128, 32), BF16)
            sc.op("dve", lambda e: e.memset(km[:], 0.0), [], [km_b])
            gt, gt_b = self.sb(es, "f_gt", (128, 32), F32)
            m8, m8_b = self.sb(es, "f_m8", (128, 8), F32)
            sb_, sb_b = self.sb(es, "f_selb", (128, 128), BF16)
            selT, selT_b = self.sb(es, "f_selT", (128, 512), BF16)
            sc.op("dve", lambda e: e.memset(selT[:], 0.0), [], [selT_b])
            b5, b5_b = self.banks[5]
        gi = 0
        for h in range(4):
            KT, KT_b = KTs[h % 2]
            Vt, Vt_b = Vts[h % 2]
            sc.dma("sp", KT[0:kd, :], self.KT[mix][h], reads=[self.bufs["KT_" + mix]], writes=[KT_b])
            sc.dma("sp", Vt[:], self.V[mix].rearrange("(n p) (h d) -> p n h d", p=128, d=65)[:, :, h, :], reads=[self.bufs["V_" + mix]], writes=[Vt_b])
            if mix == "D":
                sc.op("dve", lambda e: e.tensor_reduce(out=kmf[0:64, 0:nblk], in_=KT[0:64, :].rearrange("p (b k) -> p b k", k=256), axis=AX.X, op=ALU.add),
                      [KT_b], [kmf_b])
                sc.op("dve", lambda e: e.tensor_scalar(out=km[0:64, 0:nblk], in0=kmf[0:64, 0:nblk], scalar1=1.0 / 256, scalar2=None, op0=ALU.mult), [kmf_b], [km_b])
            for g in range(NG):
                QT, QT_b = QTs[gi % 2]
                ot, ot_b = ots[gi % 2]
                acc, acc_b = self.banks[3 + gi % 2]
                gi += 1
                sc.dma("sp", QT[0:kd, :], self.QT[mix][h, :, 512 * g:512 * (g + 1)], reads=[self.bufs["QT_" + mix]], writes=[QT_b])
                if mix == "D":
                    for j in range(4):
                        n = 4 * g + j
                        cur = n // 2
                        sc.op("pe", lambda e: e.matmul(b5[:, 0:nblk], QT[:, 128 * j:128 * (j + 1)], km[:, 0:nblk], start=True, stop=True), [QT_b, km_b], [b5_b])
                        sc.op("dve", lambda e: e.tensor_tensor(out=gt[:, 0:nblk], in0=b5[:, 0:nblk], in1=mm[:, 32 - cur:32 - cur + nblk], op=ALU.add),
                              [b5_b, mm_b], [gt_b])
                        sc.op("dve", lambda e: e.max(out=m8[:], in_=gt[:, 0:nblk]), [gt_b], [m8_b])
                        sc.op("dve", lambda e: e.tensor_scalar(out=gt[:, 0:nblk], in0=gt[:, 0:nblk], scalar1=m8[:, 2:3], scalar2=None, op0=ALU.is_ge), [gt_b, m8_b], [gt_b])
                        sc.op("dve", lambda e: e.memset(gt[:, cur:cur + 1], 1.0), [], [gt_b])
                        sc.op("dve", lambda e: e.tensor_scalar(out=sb_[:, 0:nblk], in0=gt[:, 0:nblk], scalar1=-1.0, scalar2=-NEGB, op0=ALU.add, op1=ALU.mult), [gt_b], [sb_b])
                        sv = b5.bitcast(BF16)
                        sc.op("pe", lambda e: e.transpose(sv[:, 512:640], sb_[:, :], self.ident[:]), [sb_b, self.ident_b], [b5_b])
                        sc.op("act", lambda e: e.activation(out=selT[0:32, 128 * j:128 * (j + 1)], in_=sv[0:32, 512:640], func=AF.Copy), [b5_b], [selT_b])
                kts = []
                for kt in range(4 * g + 4):
                    ex = []
                    if mix == "D":
                        ex.append((gd[:, 128 * kt:128 * (kt + 1)], selT[:], 0, 512, [gd_b, selT_b]))
                    if kt >= 4 * g:
                        ex.append((self.ident[:], mbg[:, kt - 4 * g, :], 0, 512, [self.ident_b, mbg_b]))
                    kts.append(dict(kT=KT[:, 128 * kt:128 * (kt + 1)], kb=[KT_b], extras=ex, v=[Vt[:, kt, :]] * 4, vb=[Vt_b]))
                _attn_group(self, QT, QT_b, kts, 4, acc, acc_b, scale, pts)
                _finish(self, acc, acc_b, 4, rd, rd_b)
                for j in range(4):
                    sc.op("dve", lambda e: e.tensor_scalar(out=ot[:, j, :], in0=acc[:, 128 * j:128 * j + 64], scalar1=rd[:, j:j + 1], scalar2=None, op0=ALU.mult),
                          [acc_b, rd_b], [ot_b])
                sc.dma("sp", self.O[512 * g:512 * (g + 1), col0 + 64 * h:col0 + 64 * h + 64].rearrange("(j p) d -> p j d", p=128), ot[:],
                       reads=[ot_b], writes=[self.bufs["O"]])


def phase_D1(self, l):
    sc, S, NG = self.sc, self.S, self.NG
    xsrc = self.x_in if l == 0 else self.xres
    with ExitStack() as es:
        wo, wo_b = self.sb(es, "d_wo", (128, 8, D), BF16)
        wsrc = self.w["w_out"][l].rearrange("(c p) n -> p c n", p=128)
        for c in range(8):
            self.cast_load(wo[:, c, :], wsrc[:, c, :], wo_b, D)
        xt, xt_b = self.sb(es, "d_xt", (128, 4, D), F32)
        o_, o_b = self.sb(es, "d_o", (128, 4, D), BF16)
        oT, oT_b = self.sb(es, "d_oT", (128, 8, 512), BF16)
        hT, hT_b = self.sb(es, "d_hT", (128, 8, 512), BF16)
        yt, yt_b = self.sb(es, "d_yt", (128, 512), F32)
        tmp = (*self.sb(es, "d_ss", (128, 4), F32), *self.sb(es, "d_rsd", (128, 4), F32),
               *self.sb(es, "d_xn", (128, 4, D), BF16), *self.sb(es, "d_junk", (128, D), BF16))
        for g in range(NG):
            t0 = 512 * g
            sc.dma("sp", xt[:], xsrc[t0:t0 + 512, :].rearrange("(j p) f -> p j f", p=128), reads=[self.bufs["xres"]], writes=[xt_b])
            sc.dma("sp", o_[:], self.O[t0:t0 + 512, :].rearrange("(j p) f -> p j f", p=128), reads=[self.bufs["O"]], writes=[o_b])
            for c in range(8):
                bank, bank_b = self.banks[c % 2]
                pv = bank.bitcast(BF16)
                for j in range(4):
                    sc.op("pe", lambda e: e.transpose(pv[:, 128 * j:128 * (j + 1)], o_[:, j, 128 * c:128 * (c + 1)], self.ident[:]), [o_b, self.ident_b], [bank_b])
                sc.op("act", lambda e: e.activation(out=oT[:, c, :], in_=pv[:, 0:512], func=AF.Copy), [bank_b], [oT_b])
            for j in range(4):
                for e2 in range(2):
                    bank, bank_b = self.banks[2 + e2]
                    for c in range(8):
                        sc.op("pe", lambda e: e.matmul(bank[:, :], oT[:, c, 128 * j:128 * (j + 1)], wo[:, c, 512 * e2:512 * (e2 + 1)], start=(c == 0), stop=(c == 7)),
                              [oT_b, wo_b], [bank_b])
                    sc.op("dve", lambda e: e.tensor_tensor(out=yt[:], in0=bank[:], in1=self.gb[:, 0, 512 * e2:512 * (e2 + 1)], op=ALU.mult), [bank_b, self.gb_b], [yt_b])
                    sc.op("dve", lambda e: e.tensor_tensor(out=xt[:, j, 512 * e2:512 * (e2 + 1)], in0=xt[:, j, 512 * e2:512 * (e2 + 1)], in1=yt[:], op=ALU.add),
                          [xt_b, yt_b], [xt_b])
            sc.dma("sp", self.xres[t0:t0 + 512, :].rearrange("(j p) f -> p j f", p=128), xt[:], reads=[xt_b], writes=[self.bufs["xres"]])
            self.norm_to_hT(xt, xt_b, hT, hT_b, 1, tmp)
            sc.dma("sp", self.hT2[:, :, t0:t0 + 512].rearrange("c p t -> p c t"), hT[:], reads=[hT_b], writes=[self.bufs["hT2"]])


def phase_D2(self, l, half, last):
    sc, S, NG = self.sc, self.S, self.NG
    with ExitStack() as es:
        wu, wu_b = self.sb(es, "e_wu", (128, 8, 2048), BF16)
        wd, wd_b = self.sb(es, "e_wd", (128, 16, D), BF16)
        usrc = self.w["w_up"][l].rearrange("(c p) n -> p c n", p=128)
        dsrc = self.w["w_down"][l].rearrange("(m p) n -> p m n", p=128)
        for c in range(8):
            self.cast_load(wu[:, c, :], usrc[:, c, 2048 * half:2048 * (half + 1)], wu_b, 2048)
        for m in range(16):
            self.cast_load(wd[:, m, :], dsrc[:, 16 * half + m, :], wd_b, D)
        xt, xt_b = self.sb(es, "e_xt", (128, 4, D), F32)
        hT, hT_b = self.sb(es, "e_hT", (128, 8, 512), BF16)
        uT, uT_b = self.sb(es, "e_uT", (128, 16, 512), BF16)
        r_, r_b = self.sb(es, "e_r", (128, 512), F32)
        yt, yt_b = self.sb(es, "e_yt", (128, 512), F32)
        if last:
            fg, fg_b = self.sb(es, "e_fg", (128, D), F32)
            sc.dma("sp", fg[:], self.fgb_in.broadcast_to([128, D]), writes=[fg_b])
            ss, ss_b = self.sb(es, "e_ss", (128, 4), F32)
            junk, junk_b = self.sb(es, "e_junk", (128, D), BF16)
        for g in range(NG):
            t0 = 512 * g
            sc.dma("sp", xt[:], self.xres[t0:t0 + 512, :].rearrange("(j p) f -> p j f", p=128), reads=[self.bufs["xres"]], writes=[xt_b])
            sc.dma("sp", hT[:], self.hT2[:, :, t0:t0 + 512].rearrange("c p t -> p c t"), reads=[self.bufs["hT2"]], writes=[hT_b])
            for m in range(16):
                bank, bank_b = self.banks[m % 2]
                for c in range(8):
                    sc.op("pe", lambda e: e.matmul(bank[:, :], wu[:, c, 128 * m:128 * (m + 1)], hT[:, c, :], start=(c == 0), stop=(c == 7)), [wu_b, hT_b], [bank_b])
                sc.op("act", lambda e: e.activation(out=r_[:], in_=bank[:], func=AF.Relu), [bank_b], [r_b])
                sc.op("dve", lambda e: e.tensor_tensor(out=uT[:, m, :], in0=r_[:], in1=r_[:], op=ALU.mult), [r_b], [uT_b])
            for j in range(4):
                for e2 in range(2):
                    bank, bank_b = self.banks[2 + e2]
                    for m in range(16):
                        sc.op("pe", lambda e: e.matmul(bank[:, :], uT[:, m, 128 * j:128 * (j + 1)], wd[:, m, 512 * e2:512 * (e2 + 1)], start=(m == 0), stop=(m == 15)),
                              [uT_b, wd_b], [bank_b])
                    sc.op("dve", lambda e: e.tensor_tensor(out=yt[:], in0=bank[:], in1=self.gb[:, 1, 512 * e2:512 * (e2 + 1)], op=ALU.mult), [bank_b, self.gb_b], [yt_b])
                    sc.op("dve", lambda e: e.tensor_tensor(out=xt[:, j, 512 * e2:512 * (e2 + 1)], in0=xt[:, j, 512 * e2:512 * (e2 + 1)], in1=yt[:], op=ALU.add),
                          [xt_b, yt_b], [xt_b])
            if not last:
                sc.dma("sp", self.xres[t0:t0 + 512, :].rearrange("(j p) f -> p j f", p=128), xt[:], reads=[xt_b], writes=[self.bufs["xres"]])
            else:
                for j in range(4):
                    sc.op("act", lambda e: e.activation(out=junk[:], in_=xt[:, j, :], func=AF.Square, accum_out=ss[:, j:j + 1]), [xt_b], [junk_b, ss_b])
                sc.op("act", lambda e: e.activation(out=ss[:], in_=ss[:], func=AF.Sqrt, scale=1.0 / D, bias=self.epsb[:, 0:1]), [ss_b, self.epsb_b], [ss_b])
                sc.op("dve", lambda e: e.reciprocal(out=ss[:], in_=ss[:]), [ss_b], [ss_b])
                for j in range(4):
                    sc.op("dve", lambda e: e.scalar_tensor_tensor(out=xt[:, j, :], in0=xt[:, j, :], scalar=ss[:, j:j + 1], in1=fg[:], op0=ALU.mult, op1=ALU.mult),
                          [xt_b, ss_b, fg_b], [xt_b])
                sc.dma("sp", self.out[t0:t0 + 512, :].rearrange("(j p) f -> p j f", p=128), xt[:], reads=[xt_b], writes=[self.bufs["out"]])


Prog.phase_nsa = phase_nsa
Prog.phase_cmp = phase_cmp
Prog.phase_swa = phase_swa
Prog.phase_full = phase_full
Prog.phase_D1 = phase_D1
Prog.phase_D2 = phase_D2


_CACHE = {}


def kernel(**inputs):
    x = np.asarray(inputs["x"])
    B, S, _ = x.shape
    L = np.asarray(inputs["w_in"]).shape[0]
    key = (S, L)
    if key not in _CACHE:
        _CACHE[key] = (Prog(S, L), make_consts(S))
    prog, consts = _CACHE[key]
    hw = host_weights(inputs, L)
    ncores = 8
    in_maps = [core_inputs(inputs, S, L, c % B, consts, hw) for c in range(ncores)]
    res = run_bass_kernel_spmd(prog.nc, in_maps, core_ids=list(range(ncores)))
    out = np.stack([np.asarray(res.results[b]["out"], np.float32) for b in range(B)], axis=0)
    return out
```
